# Optimizing a Trainium2 kernel written in Bass

```python
import jax, jax.numpy as jnp
from jax import lax
import numpy as np

D_MODEL = 1024
BATCH = 16
SEQ = 2048
DEPTH = 2

N_MIXERS = 2
CONV_WIDTH = D_MODEL
CONV_K = 3
N_HEADS = 16
HEAD_DIM = D_MODEL // N_HEADS
ATTN_WIDTH = N_HEADS * HEAD_DIM
MOBA_BLOCK = 256
MOBA_TOPK = 3
Q_CHUNK = 64
N_CONV_LAYERS = (DEPTH + 1) // 2
N_ATTN_LAYERS = DEPTH // 2
EPS = 1e-6
NEG = -1e30

kernel_name = "hybrid_shortconv_moba_gated"


def rms_norm(x, g):
    x32 = x.astype(jnp.float32)
    y = x32 * lax.rsqrt(jnp.mean(x32 * x32, axis=-1, keepdims=True) + EPS)
    return (y * g.astype(jnp.float32)).astype(x.dtype)


def alibi_slopes(n_heads):
    return jnp.asarray(2.0 ** (-8.0 * np.arange(1, n_heads + 1) / n_heads), dtype=jnp.float32)


def short_conv_mixer(h, w_in, conv_w, w_out):
    S = h.shape[1]
    b_gate, c_gate, xv, z = jnp.split(h @ w_in, 4, axis=-1)
    u = c_gate * xv
    u_pad = jnp.pad(u, ((0, 0), (CONV_K - 1, 0), (0, 0)))
    conv = conv_w[0] * u_pad[:, 0:S]
    for k in range(1, CONV_K):
        conv = conv + conv_w[k] * u_pad[:, k:k + S]
    y = b_gate * conv * jax.nn.silu(z)
    return y @ w_out


def moba_mixer(h, w_in, q_gain, k_gain, w_out):
    Bsz, S, _ = h.shape
    q, k, v, z = jnp.split(h @ w_in, 4, axis=-1)

    def heads(t):
        return t.reshape(Bsz, S, N_HEADS, HEAD_DIM).transpose(0, 2, 1, 3)

    q = rms_norm(heads(q), q_gain) * (HEAD_DIM ** -0.5)
    k = rms_norm(heads(k), k_gain)
    v = heads(v)

    n_blk = -(-S // MOBA_BLOCK)
    s_pad = n_blk * MOBA_BLOCK
    pad = ((0, 0), (0, 0), (0, s_pad - S), (0, 0))
    q, k, v = jnp.pad(q, pad), jnp.pad(k, pad), jnp.pad(v, pad)
    k_blocks = k.reshape(Bsz, N_HEADS, n_blk, MOBA_BLOCK, HEAD_DIM)
    v_blocks = v.reshape(Bsz, N_HEADS, n_blk, MOBA_BLOCK, HEAD_DIM)
    k_mean = jnp.mean(k_blocks.astype(jnp.float32), axis=3).astype(k.dtype)

    topk = min(MOBA_TOPK, n_blk)
    slopes = alibi_slopes(N_HEADS)
    n_chunk = s_pad // Q_CHUNK
    h_ar = jnp.arange(N_HEADS)[:, None, None]
    blk_ar = jnp.arange(n_blk)
    key_off = jnp.arange(MOBA_BLOCK)
    q_off = jnp.arange(Q_CHUNK)

    def chunk(i):
        b = i // n_chunk
        c = i % n_chunk
        t0 = c * Q_CHUNK
        qc = lax.dynamic_slice_in_dim(q[b], t0, Q_CHUNK, axis=1)
        kb = k_blocks[b]
        vb = v_blocks[b]
        q_blk = t0 // MOBA_BLOCK
        t_pos = t0 + q_off
        gate = jnp.einsum('hqd,hnd->hqn', qc, k_mean[b]).astype(jnp.float32)
        gate = jnp.where(blk_ar < q_blk, gate, NEG)
        _, idx = lax.top_k(gate, topk)
        valid = idx < q_blk
        k_sel = kb[h_ar, idx]
        v_sel = vb[h_ar, idx]
        s_past = jnp.einsum('hqd,hqjkd->hqjk', qc, k_sel).astype(jnp.float32)
        past_dist = (t_pos[None, :, None, None] - (idx[..., None] * MOBA_BLOCK + key_off)).astype(jnp.float32)
        s_past = s_past - slopes[:, None, None, None] * past_dist
        s_past = jnp.where(valid[..., None], s_past, NEG)
        k_own = lax.dynamic_index_in_dim(kb, q_blk, axis=1, keepdims=False)
        v_own = lax.dynamic_index_in_dim(vb, q_blk, axis=1, keepdims=False)
        own_dist = t_pos[:, None] - (q_blk * MOBA_BLOCK + key_off)[None, :]
        s_own = jnp.einsum('hqd,hkd->hqk', qc, k_own).astype(jnp.float32)
        s_own = jnp.where(own_dist[None] >= 0,
                          s_own - slopes[:, None, None] * own_dist.astype(jnp.float32)[None], NEG)
        n_past = topk * MOBA_BLOCK
        scores = jnp.concatenate([s_past.reshape(N_HEADS, Q_CHUNK, n_past), s_own], axis=-1)
        p = jax.nn.softmax(scores, axis=-1).astype(v.dtype)
        p_past = p[..., :n_past].reshape(N_HEADS, Q_CHUNK, topk, MOBA_BLOCK)
        p_own = p[..., n_past:]
        return (jnp.einsum('hqjk,hqjkd->hqd', p_past, v_sel)
                + jnp.einsum('hqk,hkd->hqd', p_own, v_own))

    out = lax.map(chunk, jnp.arange(Bsz * n_chunk))
    out = out.reshape(Bsz, n_chunk, N_HEADS, Q_CHUNK, HEAD_DIM).transpose(0, 1, 3, 2, 4)
    out = out.reshape(Bsz, s_pad, ATTN_WIDTH)[:, :S]
    y = out * jax.nn.silu(z)
    return y @ w_out


def setup_inputs(seed: int = 0) -> dict:
    key = jax.random.key(seed)
    ks = jax.random.split(key, 10)
    f32 = jnp.float32
    x = jax.random.normal(ks[0], (BATCH, SEQ, D_MODEL), f32)
    norm_g = 1.0 + 0.02 * jax.random.normal(ks[1], (DEPTH, D_MODEL), f32)
    conv_w_in = jax.random.normal(ks[2], (N_CONV_LAYERS, D_MODEL, 4 * CONV_WIDTH), f32) * D_MODEL ** -0.5
    conv_w = jax.random.normal(ks[3], (N_CONV_LAYERS, CONV_K, CONV_WIDTH), f32) * CONV_K ** -0.5
    conv_w_out = jax.random.normal(ks[4], (N_CONV_LAYERS, CONV_WIDTH, D_MODEL), f32) * CONV_WIDTH ** -0.5
    attn_w_in = jax.random.normal(ks[5], (N_ATTN_LAYERS, D_MODEL, 4 * ATTN_WIDTH), f32) * D_MODEL ** -0.5
    q_norm_g = 1.0 + 0.02 * jax.random.normal(ks[6], (N_ATTN_LAYERS, HEAD_DIM), f32)
    k_norm_g = 1.0 + 0.02 * jax.random.normal(ks[7], (N_ATTN_LAYERS, HEAD_DIM), f32)
    attn_w_out = jax.random.normal(ks[8], (N_ATTN_LAYERS, ATTN_WIDTH, D_MODEL), f32) * ATTN_WIDTH ** -0.5
    return {"x": x, "norm_g": norm_g, "conv_w_in": conv_w_in, "conv_w": conv_w,
            "conv_w_out": conv_w_out, "attn_w_in": attn_w_in, "q_norm_g": q_norm_g,
            "k_norm_g": k_norm_g, "attn_w_out": attn_w_out}


def reference(x, norm_g, conv_w_in, conv_w, conv_w_out, attn_w_in, q_norm_g, k_norm_g, attn_w_out):
    h = x
    for i in range(DEPTH):
        hn = rms_norm(h, norm_g[i])
        j = i // N_MIXERS
        if i % N_MIXERS == 0:
            h = h + short_conv_mixer(hn, conv_w_in[j], conv_w[j], conv_w_out[j])
        else:
            h = h + moba_mixer(hn, attn_w_in[j], q_norm_g[j], k_norm_g[j], attn_w_out[j])
    return h
```

```python
import numpy as np
from contextlib import ExitStack
import concourse.bass as bass
import concourse.mybir as mybir
from concourse.bass_utils import run_bass_kernel_spmd

F32 = mybir.dt.float32
BF16 = mybir.dt.bfloat16
ALU = mybir.AluOpType
AF = mybir.ActivationFunctionType
AX = mybir.AxisListType

D = 1024
NH = 16
HD = 64
EPS = 1e-6
BIG = 30000.0
ENGS = ("pe", "act", "dve", "pool", "sp")


class Buf:
    __slots__ = ("name", "w", "r")

    def __init__(self, name):
        self.name = name
        self.w = {}
        self.r = {}


class Ctx:
    def __init__(self, nc):
        self.nc = nc
        self.q = {e: [] for e in ENGS}
        self.cnt = {"c_" + e: 0 for e in ENGS}
        self.waited = {e: {} for e in ENGS}
        self.dma_sems = []

    def _waits(self, eng, toks):
        need = {}
        for t in toks:
            sname, val, peng = t
            if peng == eng and eng == "pe":
                continue
            if self.waited[eng].get(sname, 0) >= val:
                continue
            if need.get(sname, 0) < val:
                need[sname] = val
        for sname, val in need.items():
            self.waited[eng][sname] = val
            self.q[eng].append(("wait", sname, val))

    @staticmethod
    def _deps(reads, writes):
        toks = []
        for b in reads:
            toks.extend(b.w.values())
        for b in writes:
            toks.extend(b.w.values())
            toks.extend(b.r.values())
        return toks

    def _record(self, tok, reads, writes):
        s = tok[0]
        for b in reads:
            b.r[s] = tok
        for b in writes:
            b.w[s] = tok

    def op(self, eng, fn, reads=(), writes=()):
        self._waits(eng, self._deps(reads, writes))
        sname = "c_" + eng
        self.cnt[sname] += 1
        tok = (sname, self.cnt[sname], eng)
        self.q[eng].append(("op", fn, sname, 1))
        self._record(tok, reads, writes)
        return tok

    def dma(self, eng, fn, sem, reads=(), writes=()):
        if sem not in self.cnt:
            self.cnt[sem] = 0
            self.dma_sems.append(sem)
        self._waits(eng, self._deps(reads, writes))
        self.cnt[sem] += 16
        tok = (sem, self.cnt[sem], "dma")
        self.q[eng].append(("op", fn, sem, 16))
        self._record(tok, reads, writes)
        return tok

    def barrier(self):
        toks = [("c_" + e, self.cnt["c_" + e], e) for e in ENGS if self.cnt["c_" + e] > 0]
        toks += [(s, self.cnt[s], "dma") for s in self.dma_sems if self.cnt[s] > 0]
        for e in ENGS:
            self._waits(e, [t for t in toks if t[0] != "c_" + e])

    def replay(self):
        nc = self.nc
        with ExitStack() as st:
            sems = {}
            for sname, n in self.cnt.items():
                if n > 0:
                    sems[sname] = st.enter_context(nc.semaphore(sname))
            block = st.enter_context(nc.Block())
            engobj = {"pe": block.tensor, "act": block.scalar, "dve": block.vector,
                      "pool": block.gpsimd, "sp": block.sync}

            def make(ename):
                def body(eng):
                    for item in self.q[ename]:
                        if item[0] == "wait":
                            eng.wait_ge(sems[item[1]], item[2])
                        else:
                            _, fn, sname, inc = item
                            fn(eng).then_inc(sems[sname], inc)
                return body

            for ename in ENGS:
                if self.q[ename]:
                    engobj[ename](make(ename))


def _bf16_round(a):
    a = np.asarray(a, np.float32)
    u = a.view(np.uint32).astype(np.uint64)
    u = (u + 0x7FFF + ((u >> 16) & 1)) & 0xFFFF0000
    return u.astype(np.uint32).view(np.float32)


def make_consts(S):
    NT = S // 128
    c = {}
    c["c_ident"] = np.eye(128, dtype=np.float32)
    bd = np.zeros((128, 128), np.float32)
    bd[:64, :64] = 1.0
    bd[64:, 64:] = 1.0
    c["c_bd1"] = bd
    c["c_bd64"] = bd / 64.0
    jr = np.arange(128)[:, None]
    tr = np.arange(128)[None, :]
    c["c_tri"] = np.where(tr >= jr, 0.0, -BIG).astype(np.float32)
    j = np.arange(S)
    ka = np.zeros((16, S), np.float32)
    for n in range(8):
        ka[n] = (j // 256 == n)
    ka[8] = ka[9] = j % 256
    ka[10] = ka[11] = j // 256
    ka[12] = 1.0
    c["c_kaug"] = ka
    slopes = (2.0 ** (-8.0 * np.arange(1, NH + 1) / NH)).astype(np.float32)
    qa = np.zeros((NH, 8, S), np.float32)
    for h in range(NH):
        s_hi = _bf16_round(slopes[h])
        s_lo = _bf16_round(np.float32(slopes[h] - s_hi))
        qa[h, 0] = s_hi
        qa[h, 1] = s_lo
        qa[h, 2] = 256.0 * s_hi
        qa[h, 3] = 256.0 * s_lo
        qa[h, 4] = _bf16_round(-(slopes[h] * j.astype(np.float32)))
    c["c_qaug"] = qa
    gc = np.zeros((NT, 2, 8), np.float32)
    for tt in range(NT):
        qb = tt // 2
        for n in range(8):
            gc[tt, :, n] = 0.0 if n < qb else (1e30 if n == qb else -1e30)
    c["c_gc"] = np.broadcast_to(gc.reshape(1, -1), (128, NT * 16)).copy()
    return c


def build_program(S=2048, NSEQ=2, layers=(0, 1), NHP=8, NFC=8, NWARM1=30, NWARM2=8):
    NT = S // 128
    NQ = S // 512
    nc = bass.Bass("TRN2", target_bir_lowering=False)

    def din(name, shape):
        return nc.dram_tensor(name, list(shape), F32, kind="ExternalInput").ap()

    x_d = din("x", [NSEQ, S, D])
    out_d = nc.dram_tensor("out", [NSEQ, S, D], F32, kind="ExternalOutput").ap()
    w_in_d = [din("w_in0", [D, 4 * D]), din("w_in1", [D, 4 * D])]
    w_out_d = [din("w_out0", [D, D]), din("w_out1", [D, D])]
    gb_d = din("gb", [2, 128, D])
    cw_d = din("cw", [128, 8, 3])
    gqk_d = din("gqk", [128, 2])
    ident_d = din("c_ident", [128, 128])
    bd1_d = din("c_bd1", [128, 128])
    bd64_d = din("c_bd64", [128, 128])
    tri_d = din("c_tri", [128, 128])
    kaug_d = din("c_kaug", [16, S])
    qaug_d = din("c_qaug", [NH, 8, S])
    gc_d = din("c_gc", [128, NT * 16])

    c = Ctx(nc)

    with ExitStack() as st:
        def T(name, shape, dt):
            return st.enter_context(nc.sbuf_tensor(name, list(shape), dt))

        h = T("h", [128, NT, D], F32)
        hnT = T("hnT", [128, 8, S], BF16)
        win = [T(f"win{i}", [128, 8, 4, 128], BF16) for i in range(2)]
        gb = T("gbs", [128, D], F32)
        gc = T("gcs", [128, NT * 16], F32)
        ident = T("ident", [128, 128], BF16)
        bd1 = T("bd1", [128, 128], BF16)
        bd64 = T("bd64", [128, 128], BF16)
        tri = T("tri", [128, 128], BF16)
        cw = T("cws", [128, 8, 3], F32)
        gqk = T("gqks", [128, 2], F32)
        hn_tok = [T(f"hn_tok{i}", [128, D], BF16) for i in range(2)]
        sqj = T("sqj", [128, D], BF16)
        ssq = T("ssq", [128, NT], F32)
        nrm = T("nrm", [128, NT], F32)
        rstdN = T("rstdN", [128, NT], F32)

        ARENA = 75 * 1024
        arena = T("arena", [128, ARENA // 2], BF16)
        apos = [0]

        def carve_reset():
            apos[0] = 0

        def carve(shape_free, dt):
            n = int(np.prod(shape_free))
            nb = n * (4 if dt == F32 else 2)
            a = apos[0]
            assert a % 4 == 0
            apos[0] = a + ((nb + 3) // 4) * 4
            assert apos[0] <= ARENA, ("arena overflow", apos[0])
            v = arena[:, a // 2:(a + nb) // 2]
            if dt == F32:
                v = v.bitcast(F32)
            if len(shape_free) == 2:
                v = v.rearrange("p (a b) -> p a b", b=shape_free[1])
            elif len(shape_free) == 3:
                v = v.rearrange("p (a b c) -> p a b c", b=shape_free[1], c=shape_free[2])
            return v

        carve_reset()
        yT = carve([8, S], BF16)
        wout0 = carve([8, D], BF16)
        uf = carve([S + 2], F32)
        c_sb = [carve([512], F32) for _ in range(2)]
        sz = [carve([512], F32) for _ in range(2)]
        gg = [carve([512], F32) for _ in range(2)]
        acc = [carve([512], F32) for _ in range(2)]
        l0_bytes = apos[0]
        carve_reset()
        QA = carve([S], BF16)
        QB = carve([S], BF16)
        KA = carve([S], BF16)
        KB = carve([S], BF16)
        VA = carve([NT, 128], BF16)
        VB = carve([NT, 128], BF16)
        gz = carve([S], BF16)
        yTp2 = [carve([S], BF16) for _ in range(2)]
        pT = [carve([512], BF16) for _ in range(3)]
        sq = [carve([512], BF16) for _ in range(6)]
        lnv = [carve([512], F32) for _ in range(2)]
        rstd = lnv
        ez = [carve([512], F32) for _ in range(2)]

        rc = [carve([512], F32) for _ in range(2)]
        t1 = [carve([512], F32) for _ in range(2)]
        t1b = [carve([512], F32)] * 2
        Gt = carve([NT * 16], F32)
        M8 = carve([NT * 16], F32)
        AL = carve([NT * 16], F32)
        MB = carve([NT * 16], BF16)
        kms = carve([8], F32)
        km = carve([8], BF16)
        wout1 = [carve([D], BF16) for _ in range(4)]
        l1_bytes = apos[0]
        print("arena bytes: l0", l0_bytes, "l1", l1_bytes)

        psum = st.enter_context(nc.psum_tensor("psum", [128, 4096], F32))

        def bank(b):
            return psum[:, b * 512:(b + 1) * 512]

        def bank_bf(b, nb=1):
            return psum[:, b * 512:(b + nb) * 512].bitcast(BF16)

        B = lambda n: Buf(n)
        hB2 = [[B(f"h{t}_{k}") for k in range(2)] for t in range(NT)]
        hnTB = [B(f"hnT{t}") for t in range(NT)]
        winB = [B("win0"), B("win1")]
        constB = B("const")
        gbB = B("gb")
        hn_tokB = [B("hn_tok0"), B("hn_tok1")]
        sqjB = B("sqj")
        ssqG = [B(f"ssq{i}") for i in range((NT + 3) // 4)]
        nrmG = [B(f"nrm{i}") for i in range((NT + 3) // 4)]
        rstdG = [B(f"rstdN{i}") for i in range((NT + 3) // 4)]
        pB = [B(f"bank{i}") for i in range(8)]
        yTB = [B(f"yT{t}") for t in range(NT)]
        wout0B = B("wout0")
        ufB = [B(f"uf{t}") for t in range(NQ)]
        c_sbB, szB, ggB, accB = ([B(f"{n}{i}") for i in range(2)] for n in ("c_sb", "sz", "gg", "acc"))
        QAd = [B(f"QAd{t}") for t in range(NQ)]
        QBd = [B(f"QBd{t}") for t in range(NQ)]
        KAd = [B(f"KAd{t}") for t in range(NQ)]
        KBd = [B(f"KBd{t}") for t in range(NQ)]
        QAm, QBm, QAc, QBc, KAc, KBc = (B(n) for n in ("QAm", "QBm", "QAc", "QBc", "KAc", "KBc"))
        VAd = [B(f"VAd{t}") for t in range(NQ)]
        VBd = [B(f"VBd{t}") for t in range(NQ)]
        gzB = [B(f"gz{t}") for t in range(NQ)]
        yTpB2 = [[B(f"yTp{i}_{t}") for t in range(NQ)] for i in range(2)]
        pTB = [B(f"pT{i}") for i in range(3)]
        lnvB, ezB, rcB, t1B = ([B(f"{n}{i}") for i in range(2)] for n in ("lnv", "ez", "rc", "t1"))
        sqB = [B(f"sq{i}") for i in range(6)]
        rstdB = lnvB
        ez2B = t1B
        ez2 = t1
        t1bB = [B("t1b")] * 2
        GtB, M8B, ALB, MBB, kmsB, kmB = (B(n) for n in ("Gt", "M8", "AL", "MB", "kms", "km"))
        wout1B = [B(f"wout1_{i}") for i in range(4)]

        def MM(out, lhsT, rhs, start, stop, reads, writes):
            return c.op("pe", lambda e: e.matmul(out, lhsT=lhsT, rhs=rhs, start=start, stop=stop), reads, writes)

        def TR(out, in_, reads, writes):
            return c.op("pe", lambda e: e.transpose(out, in_, ident[:, :]), list(reads) + [constB], writes)

        def ACT(out, in_, func, reads, writes, bias=None, scale=None, accum_out=None):
            kw = {}
            if bias is not None:
                kw["bias"] = bias
            if scale is not None:
                kw["scale"] = scale
            if accum_out is not None:
                kw["accum_out"] = accum_out
            return c.op("act", lambda e: e.activation(out, in_, func, **kw), reads, writes)

        def TT(out, in0, in1, op, reads, writes, eng="dve"):
            return c.op(eng, lambda e: e.tensor_tensor(out=out, in0=in0, in1=in1, op=op), reads, writes)

        def TS(out, in0, s1, s2, op0, op1, reads, writes, eng="dve"):
            if op1 is None:
                return c.op(eng, lambda e: e.tensor_scalar(out=out, in0=in0, scalar1=s1, scalar2=None, op0=op0), reads, writes)
            return c.op(eng, lambda e: e.tensor_scalar(out=out, in0=in0, scalar1=s1, scalar2=s2, op0=op0, op1=op1), reads, writes)

        def STT(out, in0, scalar, in1, op0, op1, reads, writes):
            return c.op("dve", lambda e: e.scalar_tensor_tensor(out=out, in0=in0, scalar=scalar, in1=in1, op0=op0, op1=op1), reads, writes)

        def CP(eng, out, in_, reads, writes):
            if eng == "act":
                return c.op("act", lambda e: e.activation(out, in_, AF.Copy), reads, writes)
            return c.op(eng, lambda e: e.tensor_copy(out=out, in_=in_), reads, writes)

        def RCP(out, in_, reads, writes):
            return c.op("dve", lambda e: e.reciprocal(out=out, in_=in_), reads, writes)

        def MEMSET(eng, ap, val, writes):
            return c.op(eng, lambda e: e.memset(ap, val), (), writes)

        def DMA(eng, out, in_, sem, reads, writes):
            return c.dma(eng, lambda e: e.dma_start(out=out, in_=in_), sem, reads, writes)

        for (dst, src) in ((ident, ident_d), (bd1, bd1_d), (bd64, bd64_d), (tri, tri_d)):
            DMA("pool", dst[:, :], src, "d_setup_p", (), [constB])
        DMA("sp", gc[:, :], gc_d, "d_setup", (), [constB])
        DMA("sp", cw[:, :, :], cw_d, "d_setup", (), [constB])
        DMA("sp", gqk[:, :], gqk_d, "d_setup", (), [constB])
        c.barrier()

        units = []
        for s in range(NSEQ):
            if 0 in layers:
                units += [(s, 0, fc) for fc in range(NFC)]
            if 1 in layers:
                units += [(s, 1, hp) for hp in range(NHP)]
        unit_idx = {u: i for i, u in enumerate(units)}

        def load_unit(i):
            if i >= len(units):
                return
            s, L, j = units[i]
            slot = i % 2
            src = w_in_d[L].rearrange("(kc p) (seg f m) -> p kc seg f m", p=128, seg=4, f=8, m=128)
            for seg in range(4):
                DMA("pool", win[slot][:, :, seg, :], src[:, :, seg, j, :], f"d_win{slot}", (), [winB[slot]])
            if L == 1 and j > 0:
                DMA("pool", wout1[j % 4][:, :], w_out_d[1][j * 128:(j + 1) * 128, :], f"d_wo1_{j % 4}", (), [wout1B[j % 4]])

        def norm_phase(s, L, load_x):
            DMA("sp", gb[:, :], gb_d[L], "d_gb", (), [gbB])
            GSZ = 4
            for g0 in range(0, NT, GSZ):
                gi = g0 // GSZ
                tiles_g = list(range(g0, min(NT, g0 + GSZ)))
                gsl = slice(g0, g0 + len(tiles_g))
                for tt in tiles_g:
                    if load_x:
                        DMA("sp", h[:, tt, :], x_d[s, tt * 128:(tt + 1) * 128, :], f"d_x{tt}", (), hB2[tt])
                    if tt % 2 == 0:
                        ACT(sqj[:, :], h[:, tt, :], AF.Square, hB2[tt], [sqjB, ssqG[gi]], accum_out=ssq[:, tt:tt + 1])
                    else:
                        c.op("dve", lambda e, tt=tt: e.scalar_tensor_tensor(
                            out=hn_tok[1][:, :], in0=h[:, tt, :], scalar=1.0, in1=h[:, tt, :], op0=ALU.mult, op1=ALU.mult,
                            accum_out=ssq[:, tt:tt + 1]), hB2[tt], [hn_tokB[1], ssqG[gi]])
                TS(nrm[:, gsl], ssq[:, gsl], 1.0 / D, EPS, ALU.mult, ALU.add, [ssqG[gi]], [nrmG[gi]])
                ACT(nrm[:, gsl], nrm[:, gsl], AF.Ln, [nrmG[gi]], [nrmG[gi]])
                ACT(rstdN[:, gsl], nrm[:, gsl], AF.Exp, [nrmG[gi]], [rstdG[gi]], scale=-0.5)
                for tt in tiles_g:
                    b = tt % 2
                    STT(hn_tok[b][:, :], h[:, tt, :], rstdN[:, tt:tt + 1], gb[:, :], ALU.mult, ALU.mult,
                        hB2[tt] + [rstdG[gi], gbB], [hn_tokB[b]])
                    pb = 6 + (tt % 2)
                    pv = bank_bf(pb)
                    for kc in range(8):
                        TR(pv[:, kc * 128:(kc + 1) * 128], hn_tok[b][:, kc * 128:(kc + 1) * 128],
                           [hn_tokB[b]], [pB[pb]])
                    CP("act", hnT[:, :, tt * 128:(tt + 1) * 128], pv.rearrange("p (k t) -> p k t", t=128),
                       [pB[pb]], [hnTB[tt]])

        def layer0(s):
            MEMSET("dve", uf[:, 0:2], 0.0, [ufB[0]])
            DMA("pool", wout0[:, :, :], w_out_d[0].rearrange("(kc p) n -> p kc n", p=128), "d_wo0", (), [wout0B])
            it = 0
            for fc in range(NFC):
                ui = unit_idx[(s, 0, fc)]
                slot = ui % 2
                load_unit(ui + 1)
                w = win[slot]
                for tq in range(NQ):
                    b = it % 2
                    it += 1
                    tsl = slice(tq * 512, (tq + 1) * 512)
                    hr = [hnTB[t] for t in range(tq * 4, tq * 4 + 4)]
                    pbase = 4 * b
                    for seg in (1, 2, 3, 0):
                        pb = pbase + seg
                        for kc in range(8):
                            MM(bank(pb), w[:, kc, seg, :], hnT[:, kc, tsl], kc == 0, kc == 7,
                               [winB[slot]] + hr, [pB[pb]])
                    CP("act", c_sb[b][:, :], bank(pbase + 1), [pB[pbase + 1]], [c_sbB[b]])
                    TT(uf[:, 2 + tq * 512:2 + (tq + 1) * 512], c_sb[b][:, :], bank(pbase + 2), ALU.mult,
                       [c_sbB[b], pB[pbase + 2]], [ufB[tq]])
                    ur = [ufB[tq]] + ([ufB[tq - 1]] if tq > 0 else [])
                    t0 = tq * 512
                    TS(acc[b][:, :], uf[:, t0 + 2:t0 + 514], cw[:, fc, 2:3], None, ALU.mult, None,
                       ur + [constB], [accB[b]])
                    STT(acc[b][:, :], uf[:, t0 + 1:t0 + 513], cw[:, fc, 1:2], acc[b][:, :], ALU.mult, ALU.add,
                        ur + [constB, accB[b]], [accB[b]])
                    STT(acc[b][:, :], uf[:, t0:t0 + 512], cw[:, fc, 0:1], acc[b][:, :], ALU.mult, ALU.add,
                        ur + [constB, accB[b]], [accB[b]])
                    ACT(sz[b][:, :], bank(pbase + 3), AF.Silu, [pB[pbase + 3]], [szB[b]])
                    TT(gg[b][:, :], bank(pbase + 0), sz[b][:, :], ALU.mult, [pB[pbase + 0], szB[b]], [ggB[b]])
                    TT(yT[:, fc, tsl], acc[b][:, :], gg[b][:, :], ALU.mult, [accB[b], ggB[b]],
                       [yTB[t] for t in range(tq * 4, tq * 4 + 4)])
            it = 0
            for tt in range(NT):
                for half in range(2):
                    pb = it % 4
                    it += 1
                    for kc in range(NFC):
                        MM(bank(pb), yT[:, kc, tt * 128:(tt + 1) * 128], wout0[:, kc, half * 512:(half + 1) * 512],
                           kc == 0, kc == NFC - 1, [yTB[tt], wout0B], [pB[pb]])
                    hs = h[:, tt, half * 512:(half + 1) * 512]
                    TT(hs, hs, bank(pb), ALU.add, [hB2[tt][half], pB[pb]], [hB2[tt][half]])

        def layer1_setup():
            MEMSET("dve", QB[0:64, :], 0.0, [QBm, QBc] + QBd)
            MEMSET("dve", KB[0:64, :], 0.0, [KBc] + KBd)
            MEMSET("dve", VA[:, :, :], 1.0, VAd)
            MEMSET("dve", VB[:, :, :], 1.0, VBd)
            MEMSET("dve", kms[:, :], 0.0, [kmsB])
            r2 = lambda ap: ap.rearrange("r (a b) -> r a b", b=512)
            DMA("pool", r2(KA[64:80, :]), r2(kaug_d), "d_kaugA", (), [KAc])
            DMA("pool", r2(KB[0:16, :]), r2(kaug_d), "d_kaugB", (), [KBc])

        def layer1(s):
            layer1_setup()
            DMA("pool", wout1[0][:, :], w_out_d[1][0:128, :], "d_wo1_0", (), [wout1B[0]])
            it = 0
            oit = 0

            for hp in range(NHP):
                ui = unit_idx[(s, 1, hp)]
                slot = ui % 2
                load_unit(ui + 1)
                w = win[slot]
                r2 = lambda ap: ap.rearrange("r (a b) -> r a b", b=512)
                DMA("pool", r2(QA[72:80, :]), r2(qaug_d[2 * hp]), "d_qaugA", (), [QAc])
                DMA("pool", r2(QB[8:16, :]), r2(qaug_d[2 * hp + 1]), "d_qaugB", (), [QBc])
                def stageA(tq):
                    tsl = slice(tq * 512, (tq + 1) * 512)
                    hr = [hnTB[t] for t in range(tq * 4, tq * 4 + 4)]
                    qb_, kb_ = QKSETS[tq % 3]
                    for (seg, pb) in ((0, qb_), (1, kb_)):
                        for kc in range(8):
                            MM(bank(pb), w[:, kc, seg, :], hnT[:, kc, tsl], kc == 0, kc == 7,
                               [winB[slot]] + hr, [pB[pb]])
                    i0 = (tq % 3) * 2
                    ACT(sq[i0][:, :], bank(qb_), AF.Square, [pB[qb_]], [sqB[i0]])
                    ACT(sq[i0 + 1][:, :], bank(kb_), AF.Square, [pB[kb_]], [sqB[i0 + 1]])

                def stageB(tq):
                    tsl = slice(tq * 512, (tq + 1) * 512)
                    qb_, kb_ = QKSETS[tq % 3]
                    i0 = (tq % 3) * 2
                    MM(bank(0), bd1[:, :], sq[i0][:, :], True, True, [constB, sqB[i0]], [pB[0]])
                    MM(bank(1), bd64[:, :], sq[i0 + 1][:, :], True, True, [constB, sqB[i0 + 1]], [pB[1]])
                    ACT(lnv[0][:, :], bank(0), AF.Ln, [pB[0]], [lnvB[0]], bias=float(HD * EPS))
                    ACT(rstd[0][:, :], lnv[0][:, :], AF.Exp, [lnvB[0]], [rstdB[0]], scale=-0.5)
                    STT(QA[0:64, tsl], bank(qb_)[0:64, :], gqk[0:64, 0:1], rstd[0][0:64, :], ALU.mult, ALU.mult,
                        [pB[qb_], constB, rstdB[0]], [QAd[tq]])
                    STT(QB[64:128, tsl], bank(qb_)[64:128, :], gqk[64:128, 0:1], rstd[0][64:128, :], ALU.mult, ALU.mult,
                        [pB[qb_], constB, rstdB[0]], [QBd[tq]])
                    ACT(lnv[1][:, :], bank(1), AF.Ln, [pB[1]], [lnvB[1]], bias=float(EPS))
                    ACT(rstd[1][:, :], lnv[1][:, :], AF.Exp, [lnvB[1]], [rstdB[1]], scale=-0.5)
                    STT(KA[0:64, tsl], bank(kb_)[0:64, :], gqk[0:64, 1:2], rstd[1][0:64, :], ALU.mult, ALU.mult,
                        [pB[kb_], constB, rstdB[1]], [KAd[tq]])
                    STT(KB[64:128, tsl], bank(kb_)[64:128, :], gqk[64:128, 1:2], rstd[1][64:128, :], ALU.mult, ALU.mult,
                        [pB[kb_], constB, rstdB[1]], [KBd[tq]])
                    c.op("dve", lambda e, tq=tq, tsl=tsl: e.tensor_reduce(
                        out=kms[0:64, 2 * tq:2 * tq + 2], in_=KA[0:64, tsl].rearrange("p (n k) -> p n k", k=256),
                        axis=AX.X, op=ALU.add), [KAd[tq]], [kmsB])
                    c.op("dve", lambda e, tq=tq, tsl=tsl: e.tensor_reduce(
                        out=kms[64:128, 2 * tq:2 * tq + 2], in_=KB[64:128, tsl].rearrange("p (n k) -> p n k", k=256),
                        axis=AX.X, op=ALU.add), [KBd[tq]], [kmsB])

                QKSETS = ((5, 6), (3, 4), (7, 2))

                def stageC_pe_act(tq):
                    b = tq % 2
                    tsl = slice(tq * 512, (tq + 1) * 512)
                    hr = [hnTB[t] for t in range(tq * 4, tq * 4 + 4)]
                    zb, vb = QKSETS[(NQ + tq) % 3]
                    for kc in range(8):
                        MM(bank(zb), w[:, kc, 3, :], hnT[:, kc, tsl], kc == 0, kc == 7, [winB[slot]] + hr, [pB[zb]])
                    for t4 in range(4):
                        tt = tq * 4 + t4
                        for kc in range(8):
                            MM(bank(vb)[:, t4 * 128:(t4 + 1) * 128], hnT[:, kc, tt * 128:(tt + 1) * 128], w[:, kc, 2, :],
                               kc == 0, kc == 7, [winB[slot], hnTB[tt]], [pB[vb]])
                    ACT(ez[b][:, :], bank(zb), AF.Exp, [pB[zb]], [ezB[b]], scale=-1.0)
                    ACT(ez[b][:, :], ez[b][:, :], AF.Ln, [ezB[b]], [ezB[b]], bias=1.0)
                    ACT(ez2[b][:, :], ez[b][:, :], AF.Exp, [ezB[b]], [ez2B[b]], scale=-1.0)

                def stageC_dve(tq):
                    b = tq % 2
                    tsl = slice(tq * 512, (tq + 1) * 512)
                    zb, vb = QKSETS[(NQ + tq) % 3]
                    TT(gz[:, tsl], bank(zb), ez2[b][:, :], ALU.mult, [pB[zb], ez2B[b]], [gzB[tq]])
                    pv3 = bank(vb).rearrange("p (t c) -> p t c", c=128)
                    CP("dve", VA[:, tq * 4:(tq + 1) * 4, 0:64], pv3[:, :, 0:64], [pB[vb]], [VAd[tq]])
                    CP("dve", VB[:, tq * 4:(tq + 1) * 4, 0:64], pv3[:, :, 64:128], [pB[vb]], [VBd[tq]])

                n_early_c = 0
                for step in range(NQ + 1):
                    cnow = None
                    if step < NQ:
                        stageA(step)
                    elif step - NQ < NQ:
                        cnow = step - NQ
                        stageC_pe_act(cnow)
                        n_early_c += 1
                    if step >= 1:
                        stageB(step - 1)
                    if cnow is not None:
                        stageC_dve(cnow)
                TS(km[:, :], kms[:, :], 1.0 / 256.0, None, ALU.mult, None, [kmsB], [kmB])
                for tt in range(NT):
                    tq = tt // 4
                    for hh in range(2):
                        g = tt * 2 + hh
                        if hh == 0:
                            MM(bank(0)[:, g * 8:(g + 1) * 8], QA[0:64, tt * 128:(tt + 1) * 128], km[0:64, :],
                               True, True, [QAd[tq], kmB], [pB[0]])
                        else:
                            MM(bank(0)[:, g * 8:(g + 1) * 8], QB[64:128, tt * 128:(tt + 1) * 128], km[64:128, :],
                               True, True, [QBd[tq], kmB], [pB[0]])
                NG = NT * 2
                TT(Gt[:, :], bank(0)[:, 0:NG * 8], gc[:, :], ALU.add, [pB[0], constB], [GtB])
                late = list(range(n_early_c, NQ))
                nchunk = max(1, len(late))
                gper = (NG + nchunk - 1) // nchunk
                for ci in range(nchunk):
                    if late:
                        stageC_pe_act(late[ci])
                    for g in range(ci * gper, min(NG, (ci + 1) * gper)):
                        c.op("dve", lambda e, g=g: e.max(out=M8[:, g * 8:(g + 1) * 8], in_=Gt[:, g * 8:(g + 1) * 8]),
                             [GtB], [M8B])
                    if late:
                        stageC_dve(late[ci])
                G3 = Gt[:, :].rearrange("p (g e) -> p g e", e=8)
                thr = M8[:, :].rearrange("p (g e) -> p g e", e=8)[:, :, 3:4].to_broadcast([128, NG, 8])
                TT(AL[:, :].rearrange("p (g e) -> p g e", e=8), G3, thr, ALU.is_ge, [GtB, M8B], [ALB])
                TS(MB[:, :], AL[:, :], -1.0, BIG, ALU.add, ALU.mult, [ALB], [MBB])
                mbT = bank_bf(6, 2)
                for tt in range(NT):
                    for hh in range(2):
                        g = tt * 2 + hh
                        base = 64 if hh == 0 else 0
                        TR(mbT[base:base + 8, tt * 128:(tt + 1) * 128], MB[:, g * 8:(g + 1) * 8],
                           [MBB], [pB[6 + (tt * 128) // 1024]])
                for half in range((S + 1023) // 1024):
                    lo, hi = half * 1024, min(S, (half + 1) * 1024)
                    CP("dve", QA[64:72, lo:hi], mbT[64:72, lo:hi], [pB[6 + half]], [QAm])
                    CP("act", QB[0:8, lo:hi], mbT[0:8, lo:hi], [pB[6 + half]], [QBm])
                yTp = yTp2[hp % 2]
                yTpB = yTpB2[hp % 2]
                selA = (QA, KA, VA, slice(0, 80), QAd, QAm, QAc, KAd, KAc, VAd)
                selB = (QB, KB, VB, slice(0, 128), QBd, QBm, QBc, KBd, KBc, VBd)
                tiles = []
                for hh in range(2):
                    for qt in range(NQ):
                        nkc = 4 * qt + 4
                        for kc in range(nkc):
                            tiles.append((hh, qt, kc, nkc, it % 3))
                            it += 1
                LA = 2
                grp = {}

                def front(i):
                    hh, qt, kc, nkc, sb = tiles[i]
                    Qt, Kt, Vt, kr, Qd, Qm, Qc, Kd, Kc, Vd = selA if hh == 0 else selB
                    dd = kc - 4 * qt
                    lo = 128 * dd if dd > 0 else 0
                    MM(bank(sb)[:, lo:512], Kt[kr, kc * 128:(kc + 1) * 128],
                       Qt[kr, qt * 512 + lo:(qt + 1) * 512], True, dd < 0,
                       [Kd[kc // 4], Kc, Qd[qt], Qm, Qc], [pB[sb]])
                    if dd >= 0:
                        MM(bank(sb)[:, lo:lo + 128], ident[:, :], tri[:, :], False, True, [constB], [pB[sb]])
                    ACT(pT[sb][:, lo:512], bank(sb)[:, lo:512], AF.Exp, [pB[sb]], [pTB[sb]])

                def back(i):
                    nonlocal oit
                    hh, qt, kc, nkc, sb = tiles[i]
                    Qt, Kt, Vt, kr, Qd, Qm, Qc, Kd, Kc, Vd = selA if hh == 0 else selB
                    dd = kc - 4 * qt
                    lo = 128 * dd if dd > 0 else 0
                    if kc == 0:
                        grp[(hh, qt)] = (3 + (oit % 4), oit % 2)
                        oit += 1
                    ob, ob2 = grp[(hh, qt)]
                    MM(bank(ob)[:, lo:512], Vt[:, kc, :], pT[sb][:, lo:512], kc == 0, kc == nkc - 1,
                       [Vd[kc // 4], pTB[sb]], [pB[ob]])
                    if kc == nkc - 1:
                        qsl = slice(qt * 512, (qt + 1) * 512)
                        RCP(rc[ob2][64:128, :], bank(ob)[64:128, :], [pB[ob]], [rcB[ob2]])
                        TT(t1[ob2][0:64, :], bank(ob)[0:64, :], rc[ob2][64:128, :], ALU.mult,
                           [pB[ob], rcB[ob2]], [t1B[ob2]])
                        if hh == 0:
                            TT(yTp[0:64, qsl], t1[ob2][0:64, :], gz[0:64, qsl], ALU.mult,
                               [t1B[ob2], gzB[qt]], [yTpB[qt]], eng="pool")
                        else:
                            CP("dve", t1b[ob2][64:128, :], t1[ob2][0:64, :], [t1B[ob2]], [t1bB[ob2]])
                            TT(yTp[64:128, qsl], t1b[ob2][64:128, :], gz[64:128, qsl], ALU.mult,
                               [t1bB[ob2], gzB[qt]], [yTpB[qt]], eng="pool")

                for i in range(len(tiles) + LA):
                    if i < len(tiles):
                        front(i)
                    if i - LA >= 0:
                        back(i - LA)
                if hp % 2 == 1 or hp == NHP - 1:
                    pairs = [hp - 1, hp] if hp % 2 == 1 else [hp]
                    for tt in range(NT):
                        for half in range(2):
                            pb = 5 + (it % 3)
                            it += 1
                            for idx, ph in enumerate(pairs):
                                MM(bank(pb), yTp2[ph % 2][:, tt * 128:(tt + 1) * 128],
                                   wout1[ph % 4][:, half * 512:(half + 1) * 512],
                                   idx == 0, idx == len(pairs) - 1,
                                   [yTpB2[ph % 2][tt // 4], wout1B[ph % 4]], [pB[pb]])
                            hs = h[:, tt, half * 512:(half + 1) * 512]
                            if (tt * 2 + half) % 3 == 2:
                                sb_ = (tt * 2 + half) % 2
                                CP("act", t1[sb_][:, :], bank(pb), [pB[pb]], [t1B[sb_]])
                                TT(hs, hs, t1[sb_][:, :], ALU.add, [hB2[tt][half], t1B[sb_]], [hB2[tt][half]], eng="pool")
                            else:
                                TT(hs, hs, bank(pb), ALU.add, [hB2[tt][half], pB[pb]], [hB2[tt][half]])

        load_unit(0)
        for s in range(NSEQ):
            norm_phase(s, layers[0], load_x=True)
            for li, L in enumerate(layers):
                if L == 0:
                    layer0(s)
                else:
                    layer1(s)
                if li + 1 < len(layers):
                    norm_phase(s, layers[li + 1], load_x=False)
                    c.barrier()
            for tt in range(NT):
                DMA("sp", out_d[s, tt * 128:(tt + 1) * 128, :], h[:, tt, :], "d_out", hB2[tt], ())
            c.barrier()
        c.replay()
    return nc


def _common_inputs(norm_g, conv_w_in, conv_w, conv_w_out, attn_w_in, q_norm_g, k_norm_g, attn_w_out, S):
    f = lambda a: np.ascontiguousarray(np.asarray(a, dtype=np.float32))
    m = {
        "w_in0": f(conv_w_in[0]), "w_out0": f(conv_w_out[0]),
        "w_in1": f(attn_w_in[0]), "w_out1": f(attn_w_out[0]),
        "gb": f(np.broadcast_to(np.asarray(norm_g)[:, None, :], (2, 128, D))),
        "cw": f(np.asarray(conv_w[0]).T.reshape(8, 128, 3).transpose(1, 0, 2)),
        "gqk": f(np.stack([np.tile(np.asarray(q_norm_g[0]), 2), np.tile(np.asarray(k_norm_g[0]), 2)], axis=1)),
    }
    m.update(make_consts(S))
    return m


_PROG_CACHE = {}


def _get_prog(key, **kw):
    if key not in _PROG_CACHE:
        _PROG_CACHE[key] = build_program(**kw)
    return _PROG_CACHE[key]


FUSED = True


def kernel(x, norm_g, conv_w_in, conv_w, conv_w_out, attn_w_in, q_norm_g, k_norm_g, attn_w_out):
    x = np.asarray(x, dtype=np.float32)
    Bsz, S, _ = x.shape
    n = 8
    nseq = Bsz // n
    common = _common_inputs(norm_g, conv_w_in, conv_w, conv_w_out, attn_w_in, q_norm_g, k_norm_g, attn_w_out, S)
    shards = [np.ascontiguousarray(x[i * nseq:(i + 1) * nseq]) for i in range(n)]
    if FUSED:
        stages = [(0, 1)]
    else:
        stages = [(0,), (1,)]
    for layers in stages:
        nc = _get_prog((S, nseq, layers), S=S, NSEQ=nseq, layers=layers)
        in_maps = [dict(common, x=shards[i]) for i in range(n)]
        res = run_bass_kernel_spmd(nc, in_maps, core_ids=list(range(n)))
        shards = [np.ascontiguousarray(np.asarray(res.results[i]["out"], dtype=np.float32)) for i in range(n)]
    return np.concatenate(shards, axis=0).astype(np.float32)
```

```python
import numpy as np
from contextlib import ExitStack
import concourse.bass as bass
import concourse.mybir as mybir
from concourse.bass_utils import run_bass_kernel_spmd

F32 = mybir.dt.float32
BF16 = mybir.dt.bfloat16
ALU = mybir.AluOpType
AF = mybir.ActivationFunctionType
AX = mybir.AxisListType

D = 1024
NH = 16
HD = 64
EPS = 1e-6
BIG = 30000.0
ENGS = ("pe", "act", "dve", "pool", "sp")


class Buf:
    __slots__ = ("name", "w", "r")

    def __init__(self, name):
        self.name = name
        self.w = {}
        self.r = {}


class Ctx:
    def __init__(self, nc):
        self.nc = nc
        self.q = {e: [] for e in ENGS}
        self.cnt = {"c_" + e: 0 for e in ENGS}
        self.waited = {e: {} for e in ENGS}
        self.dma_sems = []

    def _waits(self, eng, toks):
        need = {}
        for t in toks:
            sname, val, peng = t
            if peng == eng and eng == "pe":
                continue
            if self.waited[eng].get(sname, 0) >= val:
                continue
            if need.get(sname, 0) < val:
                need[sname] = val
        for sname, val in need.items():
            self.waited[eng][sname] = val
            self.q[eng].append(("wait", sname, val))

    @staticmethod
    def _deps(reads, writes):
        toks = []
        for b in reads:
            toks.extend(b.w.values())
        for b in writes:
            toks.extend(b.w.values())
            toks.extend(b.r.values())
        return toks

    def _record(self, tok, reads, writes):
        s = tok[0]
        for b in reads:
            b.r[s] = tok
        for b in writes:
            b.w[s] = tok

    def op(self, eng, fn, reads=(), writes=()):
        self._waits(eng, self._deps(reads, writes))
        sname = "c_" + eng
        self.cnt[sname] += 1
        tok = (sname, self.cnt[sname], eng)
        self.q[eng].append(("op", fn, sname, 1))
        self._record(tok, reads, writes)
        return tok

    def dma(self, eng, fn, sem, reads=(), writes=()):
        if sem not in self.cnt:
            self.cnt[sem] = 0
            self.dma_sems.append(sem)
        self._waits(eng, self._deps(reads, writes))
        self.cnt[sem] += 16
        tok = (sem, self.cnt[sem], "dma")
        self.q[eng].append(("op", fn, sem, 16))
        self._record(tok, reads, writes)
        return tok

    def barrier(self):
        toks = [("c_" + e, self.cnt["c_" + e], e) for e in ENGS if self.cnt["c_" + e] > 0]
        toks += [(s, self.cnt[s], "dma") for s in self.dma_sems if self.cnt[s] > 0]
        for e in ENGS:
            self._waits(e, [t for t in toks if t[0] != "c_" + e])

    def replay(self):
        nc = self.nc
        with ExitStack() as st:
            sems = {}
            for sname, n in self.cnt.items():
                if n > 0:
                    sems[sname] = st.enter_context(nc.semaphore(sname))
            block = st.enter_context(nc.Block())
            engobj = {"pe": block.tensor, "act": block.scalar, "dve": block.vector,
                      "pool": block.gpsimd, "sp": block.sync}

            def make(ename):
                def body(eng):
                    for item in self.q[ename]:
                        if item[0] == "wait":
                            eng.wait_ge(sems[item[1]], item[2])
                        else:
                            _, fn, sname, inc = item
                            fn(eng).then_inc(sems[sname], inc)
                return body

            for ename in ENGS:
                if self.q[ename]:
                    engobj[ename](make(ename))


def _bf16_round(a):
    a = np.asarray(a, np.float32)
    u = a.view(np.uint32).astype(np.uint64)
    u = (u + 0x7FFF + ((u >> 16) & 1)) & 0xFFFF0000
    return u.astype(np.uint32).view(np.float32)


def make_consts(S):
    NT = S // 128
    c = {}
    c["c_ident"] = np.eye(128, dtype=np.float32)
    bd = np.zeros((128, 128), np.float32)
    bd[:64, :64] = 1.0
    bd[64:, 64:] = 1.0
    c["c_bd1"] = bd
    c["c_bd64"] = bd / 64.0
    jr = np.arange(128)[:, None]
    tr = np.arange(128)[None, :]
    c["c_tri"] = np.where(tr >= jr, 0.0, -BIG).astype(np.float32)
    j = np.arange(S)
    ka = np.zeros((16, S), np.float32)
    for n in range(8):
        ka[n] = (j // 256 == n)
    ka[8] = ka[9] = j % 256
    ka[10] = ka[11] = j // 256
    ka[12] = 1.0
    c["c_kaug"] = ka
    slopes = (2.0 ** (-8.0 * np.arange(1, NH + 1) / NH)).astype(np.float32)
    qa = np.zeros((NH, 8, S), np.float32)
    for h in range(NH):
        s_hi = _bf16_round(slopes[h])
        s_lo = _bf16_round(np.float32(slopes[h] - s_hi))
        qa[h, 0] = s_hi
        qa[h, 1] = s_lo
        qa[h, 2] = 256.0 * s_hi
        qa[h, 3] = 256.0 * s_lo
        qa[h, 4] = _bf16_round(-(slopes[h] * j.astype(np.float32)))
    c["c_qaug"] = qa
    gc = np.zeros((NT, 2, 8), np.float32)
    for tt in range(NT):
        qb = tt // 2
        for n in range(8):
            gc[tt, :, n] = 0.0 if n < qb else (1e30 if n == qb else -1e30)
    c["c_gc"] = np.broadcast_to(gc.reshape(1, -1), (128, NT * 16)).copy()
    return c


def build_program(S=2048, NSEQ=2, layers=(0, 1), NHP=8, NFC=8, NWARM1=30, NWARM2=8):
    NT = S // 128
    NQ = S // 512
    nc = bass.Bass("TRN2", target_bir_lowering=False)

    def din(name, shape):
        return nc.dram_tensor(name, list(shape), F32, kind="ExternalInput").ap()

    x_d = din("x", [NSEQ, S, D])
    out_d = nc.dram_tensor("out", [NSEQ, S, D], F32, kind="ExternalOutput").ap()
    w_in_d = [din("w_in0", [D, 4 * D]), din("w_in1", [D, 4 * D])]
    w_out_d = [din("w_out0", [D, D]), din("w_out1", [D, D])]
    gb_d = din("gb", [2, 128, D])
    cw_d = din("cw", [128, 8, 3])
    gqk_d = din("gqk", [128, 2])
    ident_d = din("c_ident", [128, 128])
    bd1_d = din("c_bd1", [128, 128])
    bd64_d = din("c_bd64", [128, 128])
    tri_d = din("c_tri", [128, 128])
    kaug_d = din("c_kaug", [16, S])
    qaug_d = din("c_qaug", [NH, 8, S])
    gc_d = din("c_gc", [128, NT * 16])

    c = Ctx(nc)

    with ExitStack() as st:
        def T(name, shape, dt):
            return st.enter_context(nc.sbuf_tensor(name, list(shape), dt))

        h = T("h", [128, NT, D], F32)
        hnT = T("hnT", [128, 8, S], BF16)
        win = [T(f"win{i}", [128, 8, 4, 128], BF16) for i in range(2)]
        gb = T("gbs", [128, D], F32)
        gc = T("gcs", [128, NT * 16], F32)
        ident = T("ident", [128, 128], BF16)
        bd1 = T("bd1", [128, 128], BF16)
        bd64 = T("bd64", [128, 128], BF16)
        tri = T("tri", [128, 128], BF16)
        cw = T("cws", [128, 8, 3], F32)
        gqk = T("gqks", [128, 2], F32)
        hn_tok = [T(f"hn_tok{i}", [128, D], BF16) for i in range(2)]
        sqj = T("sqj", [128, D], BF16)
        ssq = T("ssq", [128, NT], F32)
        nrm = T("nrm", [128, NT], F32)
        rstdN = T("rstdN", [128, NT], F32)

        ARENA = 75 * 1024
        arena = T("arena", [128, ARENA // 2], BF16)
        apos = [0]

        def carve_reset():
            apos[0] = 0

        def carve(shape_free, dt):
            n = int(np.prod(shape_free))
            nb = n * (4 if dt == F32 else 2)
            a = apos[0]
            assert a % 4 == 0
            apos[0] = a + ((nb + 3) // 4) * 4
            assert apos[0] <= ARENA, ("arena overflow", apos[0])
            v = arena[:, a // 2:(a + nb) // 2]
            if dt == F32:
                v = v.bitcast(F32)
            if len(shape_free) == 2:
                v = v.rearrange("p (a b) -> p a b", b=shape_free[1])
            elif len(shape_free) == 3:
                v = v.rearrange("p (a b c) -> p a b c", b=shape_free[1], c=shape_free[2])
            return v

        carve_reset()
        yT = carve([8, S], BF16)
        wout0 = carve([8, D], BF16)
        uf = carve([S + 2], F32)
        c_sb = [carve([512], F32) for _ in range(2)]
        sz = [carve([512], F32) for _ in range(2)]
        gg = [carve([512], F32) for _ in range(2)]
        acc = [carve([512], F32) for _ in range(2)]
        l0_bytes = apos[0]
        carve_reset()
        QA = carve([S], BF16)
        QB = carve([S], BF16)
        KA = carve([S], BF16)
        KB = carve([S], BF16)
        VA = carve([NT, 128], BF16)
        VB = carve([NT, 128], BF16)
        gz = carve([S], BF16)
        yTp2 = [carve([S], BF16) for _ in range(2)]
        pT = [carve([512], BF16) for _ in range(3)]
        sq = [carve([512], BF16) for _ in range(6)]
        lnv = [carve([512], F32) for _ in range(2)]
        rstd = lnv
        ez = [carve([512], F32) for _ in range(2)]

        rc = [carve([512], F32) for _ in range(2)]
        t1 = [carve([512], F32) for _ in range(2)]
        t1b = [carve([512], F32)] * 2
        Gt = carve([NT * 16], F32)
        M8 = carve([NT * 16], F32)
        AL = carve([NT * 16], F32)
        MB = carve([NT * 16], BF16)
        kms = carve([8], F32)
        km = carve([8], BF16)
        wout1 = [carve([D], BF16) for _ in range(4)]
        l1_bytes = apos[0]
        print("arena bytes: l0", l0_bytes, "l1", l1_bytes)

        psum = st.enter_context(nc.psum_tensor("psum", [128, 4096], F32))

        def bank(b):
            return psum[:, b * 512:(b + 1) * 512]

        def bank_bf(b, nb=1):
            return psum[:, b * 512:(b + nb) * 512].bitcast(BF16)

        B = lambda n: Buf(n)
        hB2 = [[B(f"h{t}_{k}") for k in range(2)] for t in range(NT)]
        hnTB = [B(f"hnT{t}") for t in range(NT)]
        winB = [B("win0"), B("win1")]
        constB = B("const")
        gbB = B("gb")
        hn_tokB = [B("hn_tok0"), B("hn_tok1")]
        sqjB, ssqB, nrmB, rstdNB = B("sqj"), B("ssq"), B("nrm"), B("rstdN")
        pB = [B(f"bank{i}") for i in range(8)]
        yTB = [B(f"yT{t}") for t in range(NT)]
        wout0B = B("wout0")
        ufB = [B(f"uf{t}") for t in range(NQ)]
        c_sbB, szB, ggB, accB = ([B(f"{n}{i}") for i in range(2)] for n in ("c_sb", "sz", "gg", "acc"))
        QAd = [B(f"QAd{t}") for t in range(NQ)]
        QBd = [B(f"QBd{t}") for t in range(NQ)]
        KAd = [B(f"KAd{t}") for t in range(NQ)]
        KBd = [B(f"KBd{t}") for t in range(NQ)]
        QAm, QBm, QAc, QBc, KAc, KBc = (B(n) for n in ("QAm", "QBm", "QAc", "QBc", "KAc", "KBc"))
        VAd = [B(f"VAd{t}") for t in range(NQ)]
        VBd = [B(f"VBd{t}") for t in range(NQ)]
        gzB = [B(f"gz{t}") for t in range(NQ)]
        yTpB2 = [[B(f"yTp{i}_{t}") for t in range(NQ)] for i in range(2)]
        pTB = [B(f"pT{i}") for i in range(3)]
        lnvB, ezB, rcB, t1B = ([B(f"{n}{i}") for i in range(2)] for n in ("lnv", "ez", "rc", "t1"))
        sqB = [B(f"sq{i}") for i in range(6)]
        rstdB = lnvB
        ez2B = t1B
        ez2 = t1
        t1bB = [B("t1b")] * 2
        GtB, M8B, ALB, MBB, kmsB, kmB = (B(n) for n in ("Gt", "M8", "AL", "MB", "kms", "km"))
        wout1B = [B(f"wout1_{i}") for i in range(4)]

        def MM(out, lhsT, rhs, start, stop, reads, writes):
            return c.op("pe", lambda e: e.matmul(out, lhsT=lhsT, rhs=rhs, start=start, stop=stop), reads, writes)

        def TR(out, in_, reads, writes):
            return c.op("pe", lambda e: e.transpose(out, in_, ident[:, :]), list(reads) + [constB], writes)

        def ACT(out, in_, func, reads, writes, bias=None, scale=None, accum_out=None):
            kw = {}
            if bias is not None:
                kw["bias"] = bias
            if scale is not None:
                kw["scale"] = scale
            if accum_out is not None:
                kw["accum_out"] = accum_out
            return c.op("act", lambda e: e.activation(out, in_, func, **kw), reads, writes)

        def TT(out, in0, in1, op, reads, writes, eng="dve"):
            return c.op(eng, lambda e: e.tensor_tensor(out=out, in0=in0, in1=in1, op=op), reads, writes)

        def TS(out, in0, s1, s2, op0, op1, reads, writes, eng="dve"):
            if op1 is None:
                return c.op(eng, lambda e: e.tensor_scalar(out=out, in0=in0, scalar1=s1, scalar2=None, op0=op0), reads, writes)
            return c.op(eng, lambda e: e.tensor_scalar(out=out, in0=in0, scalar1=s1, scalar2=s2, op0=op0, op1=op1), reads, writes)

        def STT(out, in0, scalar, in1, op0, op1, reads, writes):
            return c.op("dve", lambda e: e.scalar_tensor_tensor(out=out, in0=in0, scalar=scalar, in1=in1, op0=op0, op1=op1), reads, writes)

        def CP(eng, out, in_, reads, writes):
            if eng == "act":
                return c.op("act", lambda e: e.activation(out, in_, AF.Copy), reads, writes)
            return c.op(eng, lambda e: e.tensor_copy(out=out, in_=in_), reads, writes)

        def RCP(out, in_, reads, writes):
            return c.op("dve", lambda e: e.reciprocal(out=out, in_=in_), reads, writes)

        def MEMSET(eng, ap, val, writes):
            return c.op(eng, lambda e: e.memset(ap, val), (), writes)

        def DMA(eng, out, in_, sem, reads, writes):
            return c.dma(eng, lambda e: e.dma_start(out=out, in_=in_), sem, reads, writes)

        for (dst, src) in ((ident, ident_d), (bd1, bd1_d), (bd64, bd64_d), (tri, tri_d)):
            DMA("pool", dst[:, :], src, "d_setup_p", (), [constB])
        DMA("sp", gc[:, :], gc_d, "d_setup", (), [constB])
        DMA("sp", cw[:, :, :], cw_d, "d_setup", (), [constB])
        DMA("sp", gqk[:, :], gqk_d, "d_setup", (), [constB])
        c.barrier()

        units = []
        for s in range(NSEQ):
            if 0 in layers:
                units += [(s, 0, fc) for fc in range(NFC)]
            if 1 in layers:
                units += [(s, 1, hp) for hp in range(NHP)]
        unit_idx = {u: i for i, u in enumerate(units)}

        def load_unit(i):
            if i >= len(units):
                return
            s, L, j = units[i]
            slot = i % 2
            src = w_in_d[L].rearrange("(kc p) (seg f m) -> p kc seg f m", p=128, seg=4, f=8, m=128)
            for seg in range(4):
                DMA("pool", win[slot][:, :, seg, :], src[:, :, seg, j, :], f"d_win{slot}", (), [winB[slot]])
            if L == 1 and j > 0:
                DMA("pool", wout1[j % 4][:, :], w_out_d[1][j * 128:(j + 1) * 128, :], f"d_wo1_{j % 4}", (), [wout1B[j % 4]])

        def norm_phase(s, L, load_x):
            DMA("sp", gb[:, :], gb_d[L], "d_gb", (), [gbB])
            for tt in range(NT):
                if load_x:
                    DMA("sp", h[:, tt, :], x_d[s, tt * 128:(tt + 1) * 128, :], f"d_x{tt}", (), hB2[tt])
                if tt % 2 == 0:
                    ACT(sqj[:, :], h[:, tt, :], AF.Square, hB2[tt], [sqjB, ssqB], accum_out=ssq[:, tt:tt + 1])
                else:
                    c.op("dve", lambda e, tt=tt: e.scalar_tensor_tensor(
                        out=hn_tok[1][:, :], in0=h[:, tt, :], scalar=1.0, in1=h[:, tt, :], op0=ALU.mult, op1=ALU.mult,
                        accum_out=ssq[:, tt:tt + 1]), hB2[tt], [hn_tokB[1], ssqB])
            TS(nrm[:, :], ssq[:, :], 1.0 / D, EPS, ALU.mult, ALU.add, [ssqB], [nrmB])
            ACT(nrm[:, :], nrm[:, :], AF.Ln, [nrmB], [nrmB])
            ACT(rstdN[:, :], nrm[:, :], AF.Exp, [nrmB], [rstdNB], scale=-0.5)
            for tt in range(NT):
                b = tt % 2
                STT(hn_tok[b][:, :], h[:, tt, :], rstdN[:, tt:tt + 1], gb[:, :], ALU.mult, ALU.mult,
                    hB2[tt] + [rstdNB, gbB], [hn_tokB[b]])
                pb = 6 + (tt % 2)
                pv = bank_bf(pb)
                for kc in range(8):
                    TR(pv[:, kc * 128:(kc + 1) * 128], hn_tok[b][:, kc * 128:(kc + 1) * 128],
                       [hn_tokB[b]], [pB[pb]])
                CP("act", hnT[:, :, tt * 128:(tt + 1) * 128], pv.rearrange("p (k t) -> p k t", t=128),
                   [pB[pb]], [hnTB[tt]])

        def layer0(s):
            MEMSET("dve", uf[:, 0:2], 0.0, [ufB[0]])
            DMA("pool", wout0[:, :, :], w_out_d[0].rearrange("(kc p) n -> p kc n", p=128), "d_wo0", (), [wout0B])
            it = 0
            for fc in range(NFC):
                ui = unit_idx[(s, 0, fc)]
                slot = ui % 2
                load_unit(ui + 1)
                w = win[slot]
                for tq in range(NQ):
                    b = it % 2
                    it += 1
                    tsl = slice(tq * 512, (tq + 1) * 512)
                    hr = [hnTB[t] for t in range(tq * 4, tq * 4 + 4)]
                    pbase = 4 * b
                    for seg in (1, 2, 3, 0):
                        pb = pbase + seg
                        for kc in range(8):
                            MM(bank(pb), w[:, kc, seg, :], hnT[:, kc, tsl], kc == 0, kc == 7,
                               [winB[slot]] + hr, [pB[pb]])
                    CP("act", c_sb[b][:, :], bank(pbase + 1), [pB[pbase + 1]], [c_sbB[b]])
                    TT(uf[:, 2 + tq * 512:2 + (tq + 1) * 512], c_sb[b][:, :], bank(pbase + 2), ALU.mult,
                       [c_sbB[b], pB[pbase + 2]], [ufB[tq]])
                    ur = [ufB[tq]] + ([ufB[tq - 1]] if tq > 0 else [])
                    t0 = tq * 512
                    TS(acc[b][:, :], uf[:, t0 + 2:t0 + 514], cw[:, fc, 2:3], None, ALU.mult, None,
                       ur + [constB], [accB[b]])
                    STT(acc[b][:, :], uf[:, t0 + 1:t0 + 513], cw[:, fc, 1:2], acc[b][:, :], ALU.mult, ALU.add,
                        ur + [constB, accB[b]], [accB[b]])
                    STT(acc[b][:, :], uf[:, t0:t0 + 512], cw[:, fc, 0:1], acc[b][:, :], ALU.mult, ALU.add,
                        ur + [constB, accB[b]], [accB[b]])
                    ACT(sz[b][:, :], bank(pbase + 3), AF.Silu, [pB[pbase + 3]], [szB[b]])
                    TT(gg[b][:, :], bank(pbase + 0), sz[b][:, :], ALU.mult, [pB[pbase + 0], szB[b]], [ggB[b]])
                    TT(yT[:, fc, tsl], acc[b][:, :], gg[b][:, :], ALU.mult, [accB[b], ggB[b]],
                       [yTB[t] for t in range(tq * 4, tq * 4 + 4)])
            it = 0
            for tt in range(NT):
                for half in range(2):
                    pb = it % 4
                    it += 1
                    for kc in range(NFC):
                        MM(bank(pb), yT[:, kc, tt * 128:(tt + 1) * 128], wout0[:, kc, half * 512:(half + 1) * 512],
                           kc == 0, kc == NFC - 1, [yTB[tt], wout0B], [pB[pb]])
                    hs = h[:, tt, half * 512:(half + 1) * 512]
                    TT(hs, hs, bank(pb), ALU.add, [hB2[tt][half], pB[pb]], [hB2[tt][half]])

        def layer1_setup():
            MEMSET("dve", QB[0:64, :], 0.0, [QBm, QBc] + QBd)
            MEMSET("dve", KB[0:64, :], 0.0, [KBc] + KBd)
            MEMSET("dve", VA[:, :, :], 1.0, VAd)
            MEMSET("dve", VB[:, :, :], 1.0, VBd)
            MEMSET("dve", kms[:, :], 0.0, [kmsB])
            r2 = lambda ap: ap.rearrange("r (a b) -> r a b", b=512)
            DMA("pool", r2(KA[64:80, :]), r2(kaug_d), "d_kaugA", (), [KAc])
            DMA("pool", r2(KB[0:16, :]), r2(kaug_d), "d_kaugB", (), [KBc])

        def layer1(s):
            layer1_setup()
            DMA("pool", wout1[0][:, :], w_out_d[1][0:128, :], "d_wo1_0", (), [wout1B[0]])
            it = 0
            oit = 0

            for hp in range(NHP):
                ui = unit_idx[(s, 1, hp)]
                slot = ui % 2
                load_unit(ui + 1)
                w = win[slot]
                r2 = lambda ap: ap.rearrange("r (a b) -> r a b", b=512)
                DMA("pool", r2(QB[8:16, :]), r2(qaug_d[2 * hp + 1]), "d_qaugB", (), [QBc])
                def stageA(tq):
                    tsl = slice(tq * 512, (tq + 1) * 512)
                    hr = [hnTB[t] for t in range(tq * 4, tq * 4 + 4)]
                    qb_, kb_ = QKSETS[tq % 3]
                    for (seg, pb) in ((0, qb_), (1, kb_)):
                        for kc in range(8):
                            MM(bank(pb), w[:, kc, seg, :], hnT[:, kc, tsl], kc == 0, kc == 7,
                               [winB[slot]] + hr, [pB[pb]])
                    i0 = (tq % 3) * 2
                    ACT(sq[i0][:, :], bank(qb_), AF.Square, [pB[qb_]], [sqB[i0]])
                    ACT(sq[i0 + 1][:, :], bank(kb_), AF.Square, [pB[kb_]], [sqB[i0 + 1]])

                def stageB(tq):
                    tsl = slice(tq * 512, (tq + 1) * 512)
                    qb_, kb_ = QKSETS[tq % 3]
                    i0 = (tq % 3) * 2
                    MM(bank(0), bd1[:, :], sq[i0][:, :], True, True, [constB, sqB[i0]], [pB[0]])
                    MM(bank(1), bd64[:, :], sq[i0 + 1][:, :], True, True, [constB, sqB[i0 + 1]], [pB[1]])
                    ACT(lnv[0][:, :], bank(0), AF.Ln, [pB[0]], [lnvB[0]], bias=float(HD * EPS))
                    ACT(rstd[0][:, :], lnv[0][:, :], AF.Exp, [lnvB[0]], [rstdB[0]], scale=-0.5)
                    STT(QA[:, tsl], bank(qb_), gqk[:, 0:1], rstd[0][:, :], ALU.mult, ALU.mult,
                        [pB[qb_], constB, rstdB[0]], [QAd[tq], QAm, QAc])
                    CP("dve", QB[64:128, tsl], QA[64:128, tsl], [QAd[tq]], [QBd[tq]])
                    ACT(lnv[1][:, :], bank(1), AF.Ln, [pB[1]], [lnvB[1]], bias=float(EPS))
                    ACT(rstd[1][:, :], lnv[1][:, :], AF.Exp, [lnvB[1]], [rstdB[1]], scale=-0.5)
                    STT(KA[:, tsl], bank(kb_), gqk[:, 1:2], rstd[1][:, :], ALU.mult, ALU.mult,
                        [pB[kb_], constB, rstdB[1]], [KAd[tq], KAc])
                    c.op("dve", lambda e, tq=tq, tsl=tsl: e.tensor_reduce(
                        out=kms[:, 2 * tq:2 * tq + 2], in_=KA[:, tsl].rearrange("p (n k) -> p n k", k=256),
                        axis=AX.X, op=ALU.add), [KAd[tq]], [kmsB])
                    CP("dve", KB[64:128, tsl], KA[64:128, tsl], [KAd[tq]], [KBd[tq]])

                QKSETS = ((5, 6), (3, 4), (7, 2))

                def stageC_pe_act(tq):
                    b = tq % 2
                    tsl = slice(tq * 512, (tq + 1) * 512)
                    hr = [hnTB[t] for t in range(tq * 4, tq * 4 + 4)]
                    zb, vb = QKSETS[(NQ + tq) % 3]
                    for kc in range(8):
                        MM(bank(zb), w[:, kc, 3, :], hnT[:, kc, tsl], kc == 0, kc == 7, [winB[slot]] + hr, [pB[zb]])
                    for t4 in range(4):
                        tt = tq * 4 + t4
                        for kc in range(8):
                            MM(bank(vb)[:, t4 * 128:(t4 + 1) * 128], hnT[:, kc, tt * 128:(tt + 1) * 128], w[:, kc, 2, :],
                               kc == 0, kc == 7, [winB[slot], hnTB[tt]], [pB[vb]])
                    ACT(ez[b][:, :], bank(zb), AF.Exp, [pB[zb]], [ezB[b]], scale=-1.0)
                    ACT(ez[b][:, :], ez[b][:, :], AF.Ln, [ezB[b]], [ezB[b]], bias=1.0)
                    ACT(ez2[b][:, :], ez[b][:, :], AF.Exp, [ezB[b]], [ez2B[b]], scale=-1.0)

                def stageC_dve(tq):
                    b = tq % 2
                    tsl = slice(tq * 512, (tq + 1) * 512)
                    zb, vb = QKSETS[(NQ + tq) % 3]
                    TT(gz[:, tsl], bank(zb), ez2[b][:, :], ALU.mult, [pB[zb], ez2B[b]], [gzB[tq]])
                    pv3 = bank(vb).rearrange("p (t c) -> p t c", c=128)
                    CP("dve", VA[:, tq * 4:(tq + 1) * 4, 0:64], pv3[:, :, 0:64], [pB[vb]], [VAd[tq]])
                    CP("dve", VB[:, tq * 4:(tq + 1) * 4, 0:64], pv3[:, :, 64:128], [pB[vb]], [VBd[tq]])

                n_early_c = 0
                for step in range(NQ + 1):
                    cnow = None
                    if step < NQ:
                        stageA(step)
                    elif step - NQ < NQ:
                        cnow = step - NQ
                        stageC_pe_act(cnow)
                        n_early_c += 1
                    if step >= 1:
                        stageB(step - 1)
                    if cnow is not None:
                        stageC_dve(cnow)
                DMA("pool", r2(QA[72:80, :]), r2(qaug_d[2 * hp]), "d_qaugA", (), [QAc] + QAd)
                DMA("pool", r2(KA[64:80, :]), r2(kaug_d), "d_kaugA", (), [KAc] + KAd)
                TS(km[:, :], kms[:, :], 1.0 / 256.0, None, ALU.mult, None, [kmsB], [kmB])
                for tt in range(NT):
                    tq = tt // 4
                    for hh in range(2):
                        g = tt * 2 + hh
                        if hh == 0:
                            MM(bank(0)[:, g * 8:(g + 1) * 8], QA[0:64, tt * 128:(tt + 1) * 128], km[0:64, :],
                               True, True, [QAd[tq], kmB], [pB[0]])
                        else:
                            MM(bank(0)[:, g * 8:(g + 1) * 8], QB[64:128, tt * 128:(tt + 1) * 128], km[64:128, :],
                               True, True, [QBd[tq], kmB], [pB[0]])
                NG = NT * 2
                TT(Gt[:, :], bank(0)[:, 0:NG * 8], gc[:, :], ALU.add, [pB[0], constB], [GtB])
                late = list(range(n_early_c, NQ))
                nchunk = max(1, len(late))
                gper = (NG + nchunk - 1) // nchunk
                for ci in range(nchunk):
                    if late:
                        stageC_pe_act(late[ci])
                    for g in range(ci * gper, min(NG, (ci + 1) * gper)):
                        c.op("dve", lambda e, g=g: e.max(out=M8[:, g * 8:(g + 1) * 8], in_=Gt[:, g * 8:(g + 1) * 8]),
                             [GtB], [M8B])
                    if late:
                        stageC_dve(late[ci])
                G3 = Gt[:, :].rearrange("p (g e) -> p g e", e=8)
                thr = M8[:, :].rearrange("p (g e) -> p g e", e=8)[:, :, 3:4].to_broadcast([128, NG, 8])
                TT(AL[:, :].rearrange("p (g e) -> p g e", e=8), G3, thr, ALU.is_ge, [GtB, M8B], [ALB])
                TS(MB[:, :], AL[:, :], -1.0, BIG, ALU.add, ALU.mult, [ALB], [MBB])
                mbT = bank_bf(6, 2)
                for tt in range(NT):
                    for hh in range(2):
                        g = tt * 2 + hh
                        base = 64 if hh == 0 else 0
                        TR(mbT[base:base + 8, tt * 128:(tt + 1) * 128], MB[:, g * 8:(g + 1) * 8],
                           [MBB], [pB[6 + (tt * 128) // 1024]])
                for half in range((S + 1023) // 1024):
                    lo, hi = half * 1024, min(S, (half + 1) * 1024)
                    CP("dve", QA[64:72, lo:hi], mbT[64:72, lo:hi], [pB[6 + half]], [QAm])
                    CP("act", QB[0:8, lo:hi], mbT[0:8, lo:hi], [pB[6 + half]], [QBm])
                yTp = yTp2[hp % 2]
                yTpB = yTpB2[hp % 2]
                selA = (QA, KA, VA, slice(0, 80), QAd, QAm, QAc, KAd, KAc, VAd)
                selB = (QB, KB, VB, slice(0, 128), QBd, QBm, QBc, KBd, KBc, VBd)
                tiles = []
                for hh in range(2):
                    for qt in range(NQ):
                        nkc = 4 * qt + 4
                        for kc in range(nkc):
                            tiles.append((hh, qt, kc, nkc, it % 3))
                            it += 1
                LA = 2
                grp = {}

                def front(i):
                    hh, qt, kc, nkc, sb = tiles[i]
                    Qt, Kt, Vt, kr, Qd, Qm, Qc, Kd, Kc, Vd = selA if hh == 0 else selB
                    dd = kc - 4 * qt
                    lo = 128 * dd if dd > 0 else 0
                    MM(bank(sb)[:, lo:512], Kt[kr, kc * 128:(kc + 1) * 128],
                       Qt[kr, qt * 512 + lo:(qt + 1) * 512], True, dd < 0,
                       [Kd[kc // 4], Kc, Qd[qt], Qm, Qc], [pB[sb]])
                    if dd >= 0:
                        MM(bank(sb)[:, lo:lo + 128], ident[:, :], tri[:, :], False, True, [constB], [pB[sb]])
                    ACT(pT[sb][:, lo:512], bank(sb)[:, lo:512], AF.Exp, [pB[sb]], [pTB[sb]])

                def back(i):
                    nonlocal oit
                    hh, qt, kc, nkc, sb = tiles[i]
                    Qt, Kt, Vt, kr, Qd, Qm, Qc, Kd, Kc, Vd = selA if hh == 0 else selB
                    dd = kc - 4 * qt
                    lo = 128 * dd if dd > 0 else 0
                    if kc == 0:
                        grp[(hh, qt)] = (3 + (oit % 4), oit % 2)
                        oit += 1
                    ob, ob2 = grp[(hh, qt)]
                    MM(bank(ob)[:, lo:512], Vt[:, kc, :], pT[sb][:, lo:512], kc == 0, kc == nkc - 1,
                       [Vd[kc // 4], pTB[sb]], [pB[ob]])
                    if kc == nkc - 1:
                        qsl = slice(qt * 512, (qt + 1) * 512)
                        RCP(rc[ob2][64:128, :], bank(ob)[64:128, :], [pB[ob]], [rcB[ob2]])
                        TT(t1[ob2][0:64, :], bank(ob)[0:64, :], rc[ob2][64:128, :], ALU.mult,
                           [pB[ob], rcB[ob2]], [t1B[ob2]])
                        if hh == 0:
                            TT(yTp[0:64, qsl], t1[ob2][0:64, :], gz[0:64, qsl], ALU.mult,
                               [t1B[ob2], gzB[qt]], [yTpB[qt]], eng="pool")
                        else:
                            CP("dve", t1b[ob2][64:128, :], t1[ob2][0:64, :], [t1B[ob2]], [t1bB[ob2]])
                            TT(yTp[64:128, qsl], t1b[ob2][64:128, :], gz[64:128, qsl], ALU.mult,
                               [t1bB[ob2], gzB[qt]], [yTpB[qt]], eng="pool")

                for i in range(len(tiles) + LA):
                    if i < len(tiles):
                        front(i)
                    if i - LA >= 0:
                        back(i - LA)
                if hp % 2 == 1 or hp == NHP - 1:
                    pairs = [hp - 1, hp] if hp % 2 == 1 else [hp]
                    for tt in range(NT):
                        for half in range(2):
                            pb = 5 + (it % 3)
                            it += 1
                            for idx, ph in enumerate(pairs):
                                MM(bank(pb), yTp2[ph % 2][:, tt * 128:(tt + 1) * 128],
                                   wout1[ph % 4][:, half * 512:(half + 1) * 512],
                                   idx == 0, idx == len(pairs) - 1,
                                   [yTpB2[ph % 2][tt // 4], wout1B[ph % 4]], [pB[pb]])
                            hs = h[:, tt, half * 512:(half + 1) * 512]
                            if (tt * 2 + half) % 3 == 2:
                                sb_ = (tt * 2 + half) % 2
                                CP("act", t1[sb_][:, :], bank(pb), [pB[pb]], [t1B[sb_]])
                                TT(hs, hs, t1[sb_][:, :], ALU.add, [hB2[tt][half], t1B[sb_]], [hB2[tt][half]], eng="pool")
                            else:
                                TT(hs, hs, bank(pb), ALU.add, [hB2[tt][half], pB[pb]], [hB2[tt][half]])

        load_unit(0)
        for s in range(NSEQ):
            norm_phase(s, layers[0], load_x=True)
            for li, L in enumerate(layers):
                if L == 0:
                    layer0(s)
                else:
                    layer1(s)
                if li + 1 < len(layers):
                    norm_phase(s, layers[li + 1], load_x=False)
                    c.barrier()
            for tt in range(NT):
                DMA("sp", out_d[s, tt * 128:(tt + 1) * 128, :], h[:, tt, :], "d_out", hB2[tt], ())
            c.barrier()
        c.replay()
    return nc


def _common_inputs(norm_g, conv_w_in, conv_w, conv_w_out, attn_w_in, q_norm_g, k_norm_g, attn_w_out, S):
    f = lambda a: np.ascontiguousarray(np.asarray(a, dtype=np.float32))
    m = {
        "w_in0": f(conv_w_in[0]), "w_out0": f(conv_w_out[0]),
        "w_in1": f(attn_w_in[0]), "w_out1": f(attn_w_out[0]),
        "gb": f(np.broadcast_to(np.asarray(norm_g)[:, None, :], (2, 128, D))),
        "cw": f(np.asarray(conv_w[0]).T.reshape(8, 128, 3).transpose(1, 0, 2)),
        "gqk": f(np.stack([np.tile(np.asarray(q_norm_g[0]), 2), np.tile(np.asarray(k_norm_g[0]), 2)], axis=1)),
    }
    m.update(make_consts(S))
    return m


_PROG_CACHE = {}


def _get_prog(key, **kw):
    if key not in _PROG_CACHE:
        _PROG_CACHE[key] = build_program(**kw)
    return _PROG_CACHE[key]


FUSED = True


def kernel(x, norm_g, conv_w_in, conv_w, conv_w_out, attn_w_in, q_norm_g, k_norm_g, attn_w_out):
    x = np.asarray(x, dtype=np.float32)
    Bsz, S, _ = x.shape
    n = 8
    nseq = Bsz // n
    common = _common_inputs(norm_g, conv_w_in, conv_w, conv_w_out, attn_w_in, q_norm_g, k_norm_g, attn_w_out, S)
    shards = [np.ascontiguousarray(x[i * nseq:(i + 1) * nseq]) for i in range(n)]
    if FUSED:
        stages = [(0, 1)]
    else:
        stages = [(0,), (1,)]
    for layers in stages:
        nc = _get_prog((S, nseq, layers), S=S, NSEQ=nseq, layers=layers)
        in_maps = [dict(common, x=shards[i]) for i in range(n)]
        res = run_bass_kernel_spmd(nc, in_maps, core_ids=list(range(n)))
        shards = [np.ascontiguousarray(np.asarray(res.results[i]["out"], dtype=np.float32)) for i in range(n)]
    return np.concatenate(shards, axis=0).astype(np.float32)
```

```python
import numpy as np
from contextlib import ExitStack
import concourse.bass as bass
import concourse.mybir as mybir
from concourse.bass_utils import run_bass_kernel_spmd

F32 = mybir.dt.float32
BF16 = mybir.dt.bfloat16
ALU = mybir.AluOpType
AF = mybir.ActivationFunctionType
AX = mybir.AxisListType

D = 1024
NH = 16
HD = 64
EPS = 1e-6
BIG = 30000.0
ENGS = ("pe", "act", "dve", "pool", "sp")


class Buf:
    __slots__ = ("name", "w", "r")

    def __init__(self, name):
        self.name = name
        self.w = {}
        self.r = {}


class Ctx:
    def __init__(self, nc):
        self.nc = nc
        self.q = {e: [] for e in ENGS}
        self.cnt = {"c_" + e: 0 for e in ENGS}
        self.waited = {e: {} for e in ENGS}
        self.dma_sems = []

    def _waits(self, eng, toks):
        need = {}
        for t in toks:
            sname, val, peng = t
            if peng == eng and eng == "pe":
                continue
            if self.waited[eng].get(sname, 0) >= val:
                continue
            if need.get(sname, 0) < val:
                need[sname] = val
        for sname, val in need.items():
            self.waited[eng][sname] = val
            self.q[eng].append(("wait", sname, val))

    @staticmethod
    def _deps(reads, writes):
        toks = []
        for b in reads:
            toks.extend(b.w.values())
        for b in writes:
            toks.extend(b.w.values())
            toks.extend(b.r.values())
        return toks

    def _record(self, tok, reads, writes):
        s = tok[0]
        for b in reads:
            b.r[s] = tok
        for b in writes:
            b.w[s] = tok

    def op(self, eng, fn, reads=(), writes=()):
        self._waits(eng, self._deps(reads, writes))
        sname = "c_" + eng
        self.cnt[sname] += 1
        tok = (sname, self.cnt[sname], eng)
        self.q[eng].append(("op", fn, sname, 1))
        self._record(tok, reads, writes)
        return tok

    def dma(self, eng, fn, sem, reads=(), writes=()):
        if sem not in self.cnt:
            self.cnt[sem] = 0
            self.dma_sems.append(sem)
        self._waits(eng, self._deps(reads, writes))
        self.cnt[sem] += 16
        tok = (sem, self.cnt[sem], "dma")
        self.q[eng].append(("op", fn, sem, 16))
        self._record(tok, reads, writes)
        return tok

    def barrier(self):
        toks = [("c_" + e, self.cnt["c_" + e], e) for e in ENGS if self.cnt["c_" + e] > 0]
        toks += [(s, self.cnt[s], "dma") for s in self.dma_sems if self.cnt[s] > 0]
        for e in ENGS:
            self._waits(e, [t for t in toks if t[0] != "c_" + e])

    def replay(self):
        nc = self.nc
        with ExitStack() as st:
            sems = {}
            for sname, n in self.cnt.items():
                if n > 0:
                    sems[sname] = st.enter_context(nc.semaphore(sname))
            block = st.enter_context(nc.Block())
            engobj = {"pe": block.tensor, "act": block.scalar, "dve": block.vector,
                      "pool": block.gpsimd, "sp": block.sync}

            def make(ename):
                def body(eng):
                    for item in self.q[ename]:
                        if item[0] == "wait":
                            eng.wait_ge(sems[item[1]], item[2])
                        else:
                            _, fn, sname, inc = item
                            fn(eng).then_inc(sems[sname], inc)
                return body

            for ename in ENGS:
                if self.q[ename]:
                    engobj[ename](make(ename))


def _bf16_round(a):
    a = np.asarray(a, np.float32)
    u = a.view(np.uint32).astype(np.uint64)
    u = (u + 0x7FFF + ((u >> 16) & 1)) & 0xFFFF0000
    return u.astype(np.uint32).view(np.float32)


def make_consts(S):
    NT = S // 128
    c = {}
    c["c_ident"] = np.eye(128, dtype=np.float32)
    bd = np.zeros((128, 128), np.float32)
    bd[:64, :64] = 1.0
    bd[64:, 64:] = 1.0
    c["c_bd1"] = bd
    c["c_bd64"] = bd / 64.0
    jr = np.arange(128)[:, None]
    tr = np.arange(128)[None, :]
    c["c_tri"] = np.where(tr >= jr, 0.0, -BIG).astype(np.float32)
    j = np.arange(S)
    ka = np.zeros((16, S), np.float32)
    for n in range(8):
        ka[n] = (j // 256 == n)
    ka[8] = ka[9] = j % 256
    ka[10] = ka[11] = j // 256
    ka[12] = 1.0
    c["c_kaug"] = ka
    slopes = (2.0 ** (-8.0 * np.arange(1, NH + 1) / NH)).astype(np.float32)
    qa = np.zeros((NH, 8, S), np.float32)
    for h in range(NH):
        s_hi = _bf16_round(slopes[h])
        s_lo = _bf16_round(np.float32(slopes[h] - s_hi))
        qa[h, 0] = s_hi
        qa[h, 1] = s_lo
        qa[h, 2] = 256.0 * s_hi
        qa[h, 3] = 256.0 * s_lo
        qa[h, 4] = _bf16_round(-(slopes[h] * j.astype(np.float32)))
    c["c_qaug"] = qa
    gc = np.zeros((NT, 2, 8), np.float32)
    for tt in range(NT):
        qb = tt // 2
        for n in range(8):
            gc[tt, :, n] = 0.0 if n < qb else (1e30 if n == qb else -1e30)
    c["c_gc"] = np.broadcast_to(gc.reshape(1, -1), (128, NT * 16)).copy()
    return c


def build_program(S=2048, NSEQ=2, layers=(0, 1), NHP=8, NFC=8, NWARM1=30, NWARM2=8):
    NT = S // 128
    NQ = S // 512
    nc = bass.Bass("TRN2", target_bir_lowering=False)

    def din(name, shape):
        return nc.dram_tensor(name, list(shape), F32, kind="ExternalInput").ap()

    x_d = din("x", [NSEQ, S, D])
    out_d = nc.dram_tensor("out", [NSEQ, S, D], F32, kind="ExternalOutput").ap()
    w_in_d = [din("w_in0", [D, 4 * D]), din("w_in1", [D, 4 * D])]
    w_out_d = [din("w_out0", [D, D]), din("w_out1", [D, D])]
    gb_d = din("gb", [2, 128, D])
    cw_d = din("cw", [128, 8, 3])
    gqk_d = din("gqk", [128, 2])
    ident_d = din("c_ident", [128, 128])
    bd1_d = din("c_bd1", [128, 128])
    bd64_d = din("c_bd64", [128, 128])
    tri_d = din("c_tri", [128, 128])
    kaug_d = din("c_kaug", [16, S])
    qaug_d = din("c_qaug", [NH, 8, S])
    gc_d = din("c_gc", [128, NT * 16])

    c = Ctx(nc)

    with ExitStack() as st:
        def T(name, shape, dt):
            return st.enter_context(nc.sbuf_tensor(name, list(shape), dt))

        h = T("h", [128, NT, D], F32)
        hnT = T("hnT", [128, 8, S], BF16)
        win = [T(f"win{i}", [128, 8, 4, 128], BF16) for i in range(2)]
        gb = T("gbs", [128, D], F32)
        gc = T("gcs", [128, NT * 16], F32)
        ident = T("ident", [128, 128], BF16)
        bd1 = T("bd1", [128, 128], BF16)
        bd64 = T("bd64", [128, 128], BF16)
        tri = T("tri", [128, 128], BF16)
        cw = T("cws", [128, 8, 3], F32)
        gqk = T("gqks", [128, 2], F32)
        hn_tok = [T(f"hn_tok{i}", [128, D], BF16) for i in range(2)]
        sqj = T("sqj", [128, D], BF16)
        ssq = T("ssq", [128, NT], F32)
        nrm = T("nrm", [128, NT], F32)
        rstdN = T("rstdN", [128, NT], F32)

        ARENA = 76 * 1024
        arena = T("arena", [128, ARENA // 2], BF16)
        apos = [0]

        def carve_reset():
            apos[0] = 0

        def carve(shape_free, dt):
            n = int(np.prod(shape_free))
            nb = n * (4 if dt == F32 else 2)
            a = apos[0]
            assert a % 4 == 0
            apos[0] = a + ((nb + 3) // 4) * 4
            assert apos[0] <= ARENA, ("arena overflow", apos[0])
            v = arena[:, a // 2:(a + nb) // 2]
            if dt == F32:
                v = v.bitcast(F32)
            if len(shape_free) == 2:
                v = v.rearrange("p (a b) -> p a b", b=shape_free[1])
            elif len(shape_free) == 3:
                v = v.rearrange("p (a b c) -> p a b c", b=shape_free[1], c=shape_free[2])
            return v

        carve_reset()
        yT = carve([8, S], BF16)
        wout0 = carve([8, D], BF16)
        uf = carve([S + 2], F32)
        c_sb = [carve([512], F32) for _ in range(2)]
        sz = [carve([512], F32) for _ in range(2)]
        gg = [carve([512], F32) for _ in range(2)]
        acc = [carve([512], F32) for _ in range(2)]
        l0_bytes = apos[0]
        carve_reset()
        QA = carve([S], BF16)
        QB = carve([S], BF16)
        KA = carve([S], BF16)
        KB = carve([S], BF16)
        VA = carve([NT, 128], BF16)
        VB = carve([NT, 128], BF16)
        gz = carve([S], BF16)
        yTp2 = [carve([S], BF16) for _ in range(2)]
        pT = [carve([512], BF16) for _ in range(4)]
        sq = [carve([512], BF16) for _ in range(6)]
        lnv = [carve([512], F32) for _ in range(2)]
        rstd = lnv
        ez = [carve([512], F32) for _ in range(2)]

        rc = [carve([512], F32) for _ in range(2)]
        t1 = [carve([512], F32) for _ in range(2)]
        t1b = [carve([512], F32)] * 2
        Gt = carve([NT * 16], F32)
        M8 = carve([NT * 16], F32)
        AL = carve([NT * 16], F32)
        MB = carve([NT * 16], BF16)
        kms = carve([8], F32)
        km = carve([8], BF16)
        wout1 = [carve([D], BF16) for _ in range(4)]
        l1_bytes = apos[0]
        print("arena bytes: l0", l0_bytes, "l1", l1_bytes)

        psum = st.enter_context(nc.psum_tensor("psum", [128, 4096], F32))

        def bank(b):
            return psum[:, b * 512:(b + 1) * 512]

        def bank_bf(b, nb=1):
            return psum[:, b * 512:(b + nb) * 512].bitcast(BF16)

        B = lambda n: Buf(n)
        hB2 = [[B(f"h{t}_{k}") for k in range(2)] for t in range(NT)]
        hnTB = [B(f"hnT{t}") for t in range(NT)]
        winB = [B("win0"), B("win1")]
        constB = B("const")
        gbB = B("gb")
        hn_tokB = [B("hn_tok0"), B("hn_tok1")]
        sqjB, ssqB, nrmB, rstdNB = B("sqj"), B("ssq"), B("nrm"), B("rstdN")
        pB = [B(f"bank{i}") for i in range(8)]
        yTB = [B(f"yT{t}") for t in range(NT)]
        wout0B = B("wout0")
        ufB = [B(f"uf{t}") for t in range(NQ)]
        c_sbB, szB, ggB, accB = ([B(f"{n}{i}") for i in range(2)] for n in ("c_sb", "sz", "gg", "acc"))
        QAd = [B(f"QAd{t}") for t in range(NQ)]
        QBd = [B(f"QBd{t}") for t in range(NQ)]
        KAd = [B(f"KAd{t}") for t in range(NQ)]
        KBd = [B(f"KBd{t}") for t in range(NQ)]
        QAm, QBm, QAc, QBc, KAc, KBc = (B(n) for n in ("QAm", "QBm", "QAc", "QBc", "KAc", "KBc"))
        VAd = [B(f"VAd{t}") for t in range(NQ)]
        VBd = [B(f"VBd{t}") for t in range(NQ)]
        gzB = [B(f"gz{t}") for t in range(NQ)]
        yTpB2 = [[B(f"yTp{i}_{t}") for t in range(NQ)] for i in range(2)]
        pTB = [B(f"pT{i}") for i in range(4)]
        lnvB, ezB, rcB, t1B = ([B(f"{n}{i}") for i in range(2)] for n in ("lnv", "ez", "rc", "t1"))
        sqB = [B(f"sq{i}") for i in range(6)]
        rstdB = lnvB
        ez2B = t1B
        ez2 = t1
        t1bB = [B("t1b")] * 2
        GtB, M8B, ALB, MBB, kmsB, kmB = (B(n) for n in ("Gt", "M8", "AL", "MB", "kms", "km"))
        wout1B = [B(f"wout1_{i}") for i in range(4)]

        def MM(out, lhsT, rhs, start, stop, reads, writes):
            return c.op("pe", lambda e: e.matmul(out, lhsT=lhsT, rhs=rhs, start=start, stop=stop), reads, writes)

        def TR(out, in_, reads, writes):
            return c.op("pe", lambda e: e.transpose(out, in_, ident[:, :]), list(reads) + [constB], writes)

        def ACT(out, in_, func, reads, writes, bias=None, scale=None, accum_out=None):
            kw = {}
            if bias is not None:
                kw["bias"] = bias
            if scale is not None:
                kw["scale"] = scale
            if accum_out is not None:
                kw["accum_out"] = accum_out
            return c.op("act", lambda e: e.activation(out, in_, func, **kw), reads, writes)

        def TT(out, in0, in1, op, reads, writes, eng="dve"):
            return c.op(eng, lambda e: e.tensor_tensor(out=out, in0=in0, in1=in1, op=op), reads, writes)

        def TS(out, in0, s1, s2, op0, op1, reads, writes, eng="dve"):
            if op1 is None:
                return c.op(eng, lambda e: e.tensor_scalar(out=out, in0=in0, scalar1=s1, scalar2=None, op0=op0), reads, writes)
            return c.op(eng, lambda e: e.tensor_scalar(out=out, in0=in0, scalar1=s1, scalar2=s2, op0=op0, op1=op1), reads, writes)

        def STT(out, in0, scalar, in1, op0, op1, reads, writes):
            return c.op("dve", lambda e: e.scalar_tensor_tensor(out=out, in0=in0, scalar=scalar, in1=in1, op0=op0, op1=op1), reads, writes)

        def CP(eng, out, in_, reads, writes):
            if eng == "act":
                return c.op("act", lambda e: e.activation(out, in_, AF.Copy), reads, writes)
            return c.op(eng, lambda e: e.tensor_copy(out=out, in_=in_), reads, writes)

        def RCP(out, in_, reads, writes):
            return c.op("dve", lambda e: e.reciprocal(out=out, in_=in_), reads, writes)

        def MEMSET(eng, ap, val, writes):
            return c.op(eng, lambda e: e.memset(ap, val), (), writes)

        def DMA(eng, out, in_, sem, reads, writes):
            return c.dma(eng, lambda e: e.dma_start(out=out, in_=in_), sem, reads, writes)

        for (dst, src) in ((ident, ident_d), (bd1, bd1_d), (bd64, bd64_d), (tri, tri_d)):
            DMA("pool", dst[:, :], src, "d_setup_p", (), [constB])
        DMA("sp", gc[:, :], gc_d, "d_setup", (), [constB])
        DMA("sp", cw[:, :, :], cw_d, "d_setup", (), [constB])
        DMA("sp", gqk[:, :], gqk_d, "d_setup", (), [constB])
        c.barrier()

        units = []
        for s in range(NSEQ):
            if 0 in layers:
                units += [(s, 0, fc) for fc in range(NFC)]
            if 1 in layers:
                units += [(s, 1, hp) for hp in range(NHP)]
        unit_idx = {u: i for i, u in enumerate(units)}

        def load_unit(i):
            if i >= len(units):
                return
            s, L, j = units[i]
            slot = i % 2
            src = w_in_d[L].rearrange("(kc p) (seg f m) -> p kc seg f m", p=128, seg=4, f=8, m=128)
            for seg in range(4):
                DMA("pool", win[slot][:, :, seg, :], src[:, :, seg, j, :], f"d_win{slot}", (), [winB[slot]])
            if L == 1 and j > 0:
                DMA("pool", wout1[j % 4][:, :], w_out_d[1][j * 128:(j + 1) * 128, :], f"d_wo1_{j % 4}", (), [wout1B[j % 4]])

        def norm_phase(s, L, load_x):
            DMA("sp", gb[:, :], gb_d[L], "d_gb", (), [gbB])
            for tt in range(NT):
                if load_x:
                    DMA("sp", h[:, tt, :], x_d[s, tt * 128:(tt + 1) * 128, :], f"d_x{tt}", (), hB2[tt])
                if tt % 2 == 0:
                    ACT(sqj[:, :], h[:, tt, :], AF.Square, hB2[tt], [sqjB, ssqB], accum_out=ssq[:, tt:tt + 1])
                else:
                    c.op("dve", lambda e, tt=tt: e.scalar_tensor_tensor(
                        out=hn_tok[1][:, :], in0=h[:, tt, :], scalar=1.0, in1=h[:, tt, :], op0=ALU.mult, op1=ALU.mult,
                        accum_out=ssq[:, tt:tt + 1]), hB2[tt], [hn_tokB[1], ssqB])
            TS(nrm[:, :], ssq[:, :], 1.0 / D, EPS, ALU.mult, ALU.add, [ssqB], [nrmB])
            ACT(nrm[:, :], nrm[:, :], AF.Ln, [nrmB], [nrmB])
            ACT(rstdN[:, :], nrm[:, :], AF.Exp, [nrmB], [rstdNB], scale=-0.5)
            for tt in range(NT):
                b = tt % 2
                STT(hn_tok[b][:, :], h[:, tt, :], rstdN[:, tt:tt + 1], gb[:, :], ALU.mult, ALU.mult,
                    hB2[tt] + [rstdNB, gbB], [hn_tokB[b]])
                pb = 6 + (tt % 2)
                pv = bank_bf(pb)
                for kc in range(8):
                    TR(pv[:, kc * 128:(kc + 1) * 128], hn_tok[b][:, kc * 128:(kc + 1) * 128],
                       [hn_tokB[b]], [pB[pb]])
                CP("act", hnT[:, :, tt * 128:(tt + 1) * 128], pv.rearrange("p (k t) -> p k t", t=128),
                   [pB[pb]], [hnTB[tt]])

        def layer0(s):
            MEMSET("dve", uf[:, 0:2], 0.0, [ufB[0]])
            DMA("pool", wout0[:, :, :], w_out_d[0].rearrange("(kc p) n -> p kc n", p=128), "d_wo0", (), [wout0B])
            it = 0
            for fc in range(NFC):
                ui = unit_idx[(s, 0, fc)]
                slot = ui % 2
                load_unit(ui + 1)
                w = win[slot]
                for tq in range(NQ):
                    b = it % 2
                    it += 1
                    tsl = slice(tq * 512, (tq + 1) * 512)
                    hr = [hnTB[t] for t in range(tq * 4, tq * 4 + 4)]
                    pbase = 4 * b
                    for seg in (1, 2, 3, 0):
                        pb = pbase + seg
                        for kc in range(8):
                            MM(bank(pb), w[:, kc, seg, :], hnT[:, kc, tsl], kc == 0, kc == 7,
                               [winB[slot]] + hr, [pB[pb]])
                    CP("act", c_sb[b][:, :], bank(pbase + 1), [pB[pbase + 1]], [c_sbB[b]])
                    TT(uf[:, 2 + tq * 512:2 + (tq + 1) * 512], c_sb[b][:, :], bank(pbase + 2), ALU.mult,
                       [c_sbB[b], pB[pbase + 2]], [ufB[tq]])
                    ur = [ufB[tq]] + ([ufB[tq - 1]] if tq > 0 else [])
                    t0 = tq * 512
                    TS(acc[b][:, :], uf[:, t0 + 2:t0 + 514], cw[:, fc, 2:3], None, ALU.mult, None,
                       ur + [constB], [accB[b]])
                    STT(acc[b][:, :], uf[:, t0 + 1:t0 + 513], cw[:, fc, 1:2], acc[b][:, :], ALU.mult, ALU.add,
                        ur + [constB, accB[b]], [accB[b]])
                    STT(acc[b][:, :], uf[:, t0:t0 + 512], cw[:, fc, 0:1], acc[b][:, :], ALU.mult, ALU.add,
                        ur + [constB, accB[b]], [accB[b]])
                    ACT(sz[b][:, :], bank(pbase + 3), AF.Silu, [pB[pbase + 3]], [szB[b]])
                    TT(gg[b][:, :], bank(pbase + 0), sz[b][:, :], ALU.mult, [pB[pbase + 0], szB[b]], [ggB[b]])
                    TT(yT[:, fc, tsl], acc[b][:, :], gg[b][:, :], ALU.mult, [accB[b], ggB[b]],
                       [yTB[t] for t in range(tq * 4, tq * 4 + 4)])
            it = 0
            for tt in range(NT):
                for half in range(2):
                    pb = it % 4
                    it += 1
                    for kc in range(NFC):
                        MM(bank(pb), yT[:, kc, tt * 128:(tt + 1) * 128], wout0[:, kc, half * 512:(half + 1) * 512],
                           kc == 0, kc == NFC - 1, [yTB[tt], wout0B], [pB[pb]])
                    hs = h[:, tt, half * 512:(half + 1) * 512]
                    TT(hs, hs, bank(pb), ALU.add, [hB2[tt][half], pB[pb]], [hB2[tt][half]])

        def layer1_setup():
            MEMSET("dve", QB[0:64, :], 0.0, [QBm, QBc] + QBd)
            MEMSET("dve", KB[0:64, :], 0.0, [KBc] + KBd)
            MEMSET("dve", VA[:, :, :], 1.0, VAd)
            MEMSET("dve", VB[:, :, :], 1.0, VBd)
            MEMSET("dve", kms[:, :], 0.0, [kmsB])
            r2 = lambda ap: ap.rearrange("r (a b) -> r a b", b=512)
            DMA("pool", r2(KA[64:80, :]), r2(kaug_d), "d_kaugA", (), [KAc])
            DMA("pool", r2(KB[0:16, :]), r2(kaug_d), "d_kaugB", (), [KBc])

        def layer1(s):
            layer1_setup()
            DMA("pool", wout1[0][:, :], w_out_d[1][0:128, :], "d_wo1_0", (), [wout1B[0]])
            it = 0
            oit = 0

            for hp in range(NHP):
                ui = unit_idx[(s, 1, hp)]
                slot = ui % 2
                load_unit(ui + 1)
                w = win[slot]
                r2 = lambda ap: ap.rearrange("r (a b) -> r a b", b=512)
                DMA("pool", r2(QB[8:16, :]), r2(qaug_d[2 * hp + 1]), "d_qaugB", (), [QBc])
                def stageA(tq):
                    tsl = slice(tq * 512, (tq + 1) * 512)
                    hr = [hnTB[t] for t in range(tq * 4, tq * 4 + 4)]
                    qb_, kb_ = QKSETS[tq % 3]
                    for (seg, pb) in ((0, qb_), (1, kb_)):
                        for kc in range(8):
                            MM(bank(pb), w[:, kc, seg, :], hnT[:, kc, tsl], kc == 0, kc == 7,
                               [winB[slot]] + hr, [pB[pb]])
                    i0 = (tq % 3) * 2
                    ACT(sq[i0][:, :], bank(qb_), AF.Square, [pB[qb_]], [sqB[i0]])
                    ACT(sq[i0 + 1][:, :], bank(kb_), AF.Square, [pB[kb_]], [sqB[i0 + 1]])

                def stageB(tq):
                    tsl = slice(tq * 512, (tq + 1) * 512)
                    qb_, kb_ = QKSETS[tq % 3]
                    i0 = (tq % 3) * 2
                    MM(bank(0), bd1[:, :], sq[i0][:, :], True, True, [constB, sqB[i0]], [pB[0]])
                    MM(bank(1), bd64[:, :], sq[i0 + 1][:, :], True, True, [constB, sqB[i0 + 1]], [pB[1]])
                    ACT(lnv[0][:, :], bank(0), AF.Ln, [pB[0]], [lnvB[0]], bias=float(HD * EPS))
                    ACT(rstd[0][:, :], lnv[0][:, :], AF.Exp, [lnvB[0]], [rstdB[0]], scale=-0.5)
                    STT(QA[:, tsl], bank(qb_), gqk[:, 0:1], rstd[0][:, :], ALU.mult, ALU.mult,
                        [pB[qb_], constB, rstdB[0]], [QAd[tq], QAm, QAc])
                    CP("dve", QB[64:128, tsl], QA[64:128, tsl], [QAd[tq]], [QBd[tq]])
                    ACT(lnv[1][:, :], bank(1), AF.Ln, [pB[1]], [lnvB[1]], bias=float(EPS))
                    ACT(rstd[1][:, :], lnv[1][:, :], AF.Exp, [lnvB[1]], [rstdB[1]], scale=-0.5)
                    STT(KA[:, tsl], bank(kb_), gqk[:, 1:2], rstd[1][:, :], ALU.mult, ALU.mult,
                        [pB[kb_], constB, rstdB[1]], [KAd[tq], KAc])
                    c.op("dve", lambda e, tq=tq, tsl=tsl: e.tensor_reduce(
                        out=kms[:, 2 * tq:2 * tq + 2], in_=KA[:, tsl].rearrange("p (n k) -> p n k", k=256),
                        axis=AX.X, op=ALU.add), [KAd[tq]], [kmsB])
                    CP("dve", KB[64:128, tsl], KA[64:128, tsl], [KAd[tq]], [KBd[tq]])

                QKSETS = ((5, 6), (3, 4), (7, 2))

                def stageC_pe_act(tq):
                    b = tq % 2
                    tsl = slice(tq * 512, (tq + 1) * 512)
                    hr = [hnTB[t] for t in range(tq * 4, tq * 4 + 4)]
                    zb, vb = QKSETS[(NQ + tq) % 3]
                    for kc in range(8):
                        MM(bank(zb), w[:, kc, 3, :], hnT[:, kc, tsl], kc == 0, kc == 7, [winB[slot]] + hr, [pB[zb]])
                    for t4 in range(4):
                        tt = tq * 4 + t4
                        for kc in range(8):
                            MM(bank(vb)[:, t4 * 128:(t4 + 1) * 128], hnT[:, kc, tt * 128:(tt + 1) * 128], w[:, kc, 2, :],
                               kc == 0, kc == 7, [winB[slot], hnTB[tt]], [pB[vb]])
                    ACT(ez[b][:, :], bank(zb), AF.Exp, [pB[zb]], [ezB[b]], scale=-1.0)
                    ACT(ez[b][:, :], ez[b][:, :], AF.Ln, [ezB[b]], [ezB[b]], bias=1.0)
                    ACT(ez2[b][:, :], ez[b][:, :], AF.Exp, [ezB[b]], [ez2B[b]], scale=-1.0)

                def stageC_dve(tq):
                    b = tq % 2
                    tsl = slice(tq * 512, (tq + 1) * 512)
                    zb, vb = QKSETS[(NQ + tq) % 3]
                    TT(gz[:, tsl], bank(zb), ez2[b][:, :], ALU.mult, [pB[zb], ez2B[b]], [gzB[tq]])
                    pv3 = bank(vb).rearrange("p (t c) -> p t c", c=128)
                    CP("dve", VA[:, tq * 4:(tq + 1) * 4, 0:64], pv3[:, :, 0:64], [pB[vb]], [VAd[tq]])
                    CP("dve", VB[:, tq * 4:(tq + 1) * 4, 0:64], pv3[:, :, 64:128], [pB[vb]], [VBd[tq]])

                n_early_c = 0
                for step in range(NQ + 1):
                    cnow = None
                    if step < NQ:
                        stageA(step)
                    elif step - NQ < NQ:
                        cnow = step - NQ
                        stageC_pe_act(cnow)
                        n_early_c += 1
                    if step >= 1:
                        stageB(step - 1)
                    if cnow is not None:
                        stageC_dve(cnow)
                DMA("pool", r2(QA[72:80, :]), r2(qaug_d[2 * hp]), "d_qaugA", (), [QAc] + QAd)
                DMA("pool", r2(KA[64:80, :]), r2(kaug_d), "d_kaugA", (), [KAc] + KAd)
                TS(km[:, :], kms[:, :], 1.0 / 256.0, None, ALU.mult, None, [kmsB], [kmB])
                for tt in range(NT):
                    tq = tt // 4
                    for hh in range(2):
                        g = tt * 2 + hh
                        if hh == 0:
                            MM(bank(0)[:, g * 8:(g + 1) * 8], QA[0:64, tt * 128:(tt + 1) * 128], km[0:64, :],
                               True, True, [QAd[tq], kmB], [pB[0]])
                        else:
                            MM(bank(0)[:, g * 8:(g + 1) * 8], QB[64:128, tt * 128:(tt + 1) * 128], km[64:128, :],
                               True, True, [QBd[tq], kmB], [pB[0]])
                NG = NT * 2
                TT(Gt[:, :], bank(0)[:, 0:NG * 8], gc[:, :], ALU.add, [pB[0], constB], [GtB])
                late = list(range(n_early_c, NQ))
                nchunk = max(1, len(late))
                gper = (NG + nchunk - 1) // nchunk
                for ci in range(nchunk):
                    if late:
                        stageC_pe_act(late[ci])
                    for g in range(ci * gper, min(NG, (ci + 1) * gper)):
                        c.op("dve", lambda e, g=g: e.max(out=M8[:, g * 8:(g + 1) * 8], in_=Gt[:, g * 8:(g + 1) * 8]),
                             [GtB], [M8B])
                    if late:
                        stageC_dve(late[ci])
                G3 = Gt[:, :].rearrange("p (g e) -> p g e", e=8)
                thr = M8[:, :].rearrange("p (g e) -> p g e", e=8)[:, :, 3:4].to_broadcast([128, NG, 8])
                TT(AL[:, :].rearrange("p (g e) -> p g e", e=8), G3, thr, ALU.is_ge, [GtB, M8B], [ALB])
                TS(MB[:, :], AL[:, :], -1.0, BIG, ALU.add, ALU.mult, [ALB], [MBB])
                mbT = bank_bf(6, 2)
                for tt in range(NT):
                    for hh in range(2):
                        g = tt * 2 + hh
                        base = 64 if hh == 0 else 0
                        TR(mbT[base:base + 8, tt * 128:(tt + 1) * 128], MB[:, g * 8:(g + 1) * 8],
                           [MBB], [pB[6 + (tt * 128) // 1024]])
                for half in range((S + 1023) // 1024):
                    lo, hi = half * 1024, min(S, (half + 1) * 1024)
                    CP("dve", QA[64:72, lo:hi], mbT[64:72, lo:hi], [pB[6 + half]], [QAm])
                    CP("act", QB[0:8, lo:hi], mbT[0:8, lo:hi], [pB[6 + half]], [QBm])
                yTp = yTp2[hp % 2]
                yTpB = yTpB2[hp % 2]
                selA = (QA, KA, VA, slice(0, 80), QAd, QAm, QAc, KAd, KAc, VAd)
                selB = (QB, KB, VB, slice(0, 128), QBd, QBm, QBc, KBd, KBc, VBd)
                tiles = []
                for hh in range(2):
                    for qt in range(NQ):
                        nkc = 4 * qt + 4
                        for kc in range(nkc):
                            tiles.append((hh, qt, kc, nkc, it % 4))
                            it += 1
                LA = 3
                SBANK = (0, 1, 2, 7)
                grp = {}

                def front(i):
                    hh, qt, kc, nkc, sb = tiles[i]
                    Qt, Kt, Vt, kr, Qd, Qm, Qc, Kd, Kc, Vd = selA if hh == 0 else selB
                    dd = kc - 4 * qt
                    lo = 128 * dd if dd > 0 else 0
                    bk = SBANK[sb]
                    MM(bank(bk)[:, lo:512], Kt[kr, kc * 128:(kc + 1) * 128],
                       Qt[kr, qt * 512 + lo:(qt + 1) * 512], True, dd < 0,
                       [Kd[kc // 4], Kc, Qd[qt], Qm, Qc], [pB[bk]])
                    if dd >= 0:
                        MM(bank(bk)[:, lo:lo + 128], ident[:, :], tri[:, :], False, True, [constB], [pB[bk]])
                    ACT(pT[sb][:, lo:512], bank(bk)[:, lo:512], AF.Exp, [pB[bk]], [pTB[sb]])

                def back(i):
                    nonlocal oit
                    hh, qt, kc, nkc, sb = tiles[i]
                    Qt, Kt, Vt, kr, Qd, Qm, Qc, Kd, Kc, Vd = selA if hh == 0 else selB
                    dd = kc - 4 * qt
                    lo = 128 * dd if dd > 0 else 0
                    if kc == 0:
                        grp[(hh, qt)] = (3 + (oit % 4), oit % 2)
                        oit += 1
                    ob, ob2 = grp[(hh, qt)]
                    MM(bank(ob)[:, lo:512], Vt[:, kc, :], pT[sb][:, lo:512], kc == 0, kc == nkc - 1,
                       [Vd[kc // 4], pTB[sb]], [pB[ob]])
                    if kc == nkc - 1:
                        qsl = slice(qt * 512, (qt + 1) * 512)
                        RCP(rc[ob2][64:128, :], bank(ob)[64:128, :], [pB[ob]], [rcB[ob2]])
                        TT(t1[ob2][0:64, :], bank(ob)[0:64, :], rc[ob2][64:128, :], ALU.mult,
                           [pB[ob], rcB[ob2]], [t1B[ob2]])
                        if hh == 0:
                            TT(yTp[0:64, qsl], t1[ob2][0:64, :], gz[0:64, qsl], ALU.mult,
                               [t1B[ob2], gzB[qt]], [yTpB[qt]], eng="pool")
                        else:
                            CP("dve", t1b[ob2][64:128, :], t1[ob2][0:64, :], [t1B[ob2]], [t1bB[ob2]])
                            TT(yTp[64:128, qsl], t1b[ob2][64:128, :], gz[64:128, qsl], ALU.mult,
                               [t1bB[ob2], gzB[qt]], [yTpB[qt]], eng="pool")

                for i in range(len(tiles) + LA):
                    if i < len(tiles):
                        front(i)
                    if i - LA >= 0:
                        back(i - LA)
                if hp % 2 == 1 or hp == NHP - 1:
                    pairs = [hp - 1, hp] if hp % 2 == 1 else [hp]
                    for tt in range(NT):
                        for half in range(2):
                            pb = 5 + (it % 3)
                            it += 1
                            for idx, ph in enumerate(pairs):
                                MM(bank(pb), yTp2[ph % 2][:, tt * 128:(tt + 1) * 128],
                                   wout1[ph % 4][:, half * 512:(half + 1) * 512],
                                   idx == 0, idx == len(pairs) - 1,
                                   [yTpB2[ph % 2][tt // 4], wout1B[ph % 4]], [pB[pb]])
                            hs = h[:, tt, half * 512:(half + 1) * 512]
                            if (tt * 2 + half) % 3 == 2:
                                sb_ = (tt * 2 + half) % 2
                                CP("act", t1[sb_][:, :], bank(pb), [pB[pb]], [t1B[sb_]])
                                TT(hs, hs, t1[sb_][:, :], ALU.add, [hB2[tt][half], t1B[sb_]], [hB2[tt][half]], eng="pool")
                            else:
                                TT(hs, hs, bank(pb), ALU.add, [hB2[tt][half], pB[pb]], [hB2[tt][half]])

        load_unit(0)
        for s in range(NSEQ):
            norm_phase(s, layers[0], load_x=True)
            for li, L in enumerate(layers):
                if L == 0:
                    layer0(s)
                else:
                    layer1(s)
                if li + 1 < len(layers):
                    norm_phase(s, layers[li + 1], load_x=False)
                    c.barrier()
            for tt in range(NT):
                DMA("sp", out_d[s, tt * 128:(tt + 1) * 128, :], h[:, tt, :], "d_out", hB2[tt], ())
            c.barrier()
        c.replay()
    return nc


def _common_inputs(norm_g, conv_w_in, conv_w, conv_w_out, attn_w_in, q_norm_g, k_norm_g, attn_w_out, S):
    f = lambda a: np.ascontiguousarray(np.asarray(a, dtype=np.float32))
    m = {
        "w_in0": f(conv_w_in[0]), "w_out0": f(conv_w_out[0]),
        "w_in1": f(attn_w_in[0]), "w_out1": f(attn_w_out[0]),
        "gb": f(np.broadcast_to(np.asarray(norm_g)[:, None, :], (2, 128, D))),
        "cw": f(np.asarray(conv_w[0]).T.reshape(8, 128, 3).transpose(1, 0, 2)),
        "gqk": f(np.stack([np.tile(np.asarray(q_norm_g[0]), 2), np.tile(np.asarray(k_norm_g[0]), 2)], axis=1)),
    }
    m.update(make_consts(S))
    return m


_PROG_CACHE = {}


def _get_prog(key, **kw):
    if key not in _PROG_CACHE:
        _PROG_CACHE[key] = build_program(**kw)
    return _PROG_CACHE[key]


FUSED = True


def kernel(x, norm_g, conv_w_in, conv_w, conv_w_out, attn_w_in, q_norm_g, k_norm_g, attn_w_out):
    x = np.asarray(x, dtype=np.float32)
    Bsz, S, _ = x.shape
    n = 8
    nseq = Bsz // n
    common = _common_inputs(norm_g, conv_w_in, conv_w, conv_w_out, attn_w_in, q_norm_g, k_norm_g, attn_w_out, S)
    shards = [np.ascontiguousarray(x[i * nseq:(i + 1) * nseq]) for i in range(n)]
    if FUSED:
        stages = [(0, 1)]
    else:
        stages = [(0,), (1,)]
    for layers in stages:
        nc = _get_prog((S, nseq, layers), S=S, NSEQ=nseq, layers=layers)
        in_maps = [dict(common, x=shards[i]) for i in range(n)]
        res = run_bass_kernel_spmd(nc, in_maps, core_ids=list(range(n)))
        shards = [np.ascontiguousarray(np.asarray(res.results[i]["out"], dtype=np.float32)) for i in range(n)]
    return np.concatenate(shards, axis=0).astype(np.float32)
```

```python
import numpy as np
from contextlib import ExitStack
import concourse.bass as bass
import concourse.mybir as mybir
from concourse.bass_utils import run_bass_kernel_spmd

F32 = mybir.dt.float32
BF16 = mybir.dt.bfloat16
ALU = mybir.AluOpType
AF = mybir.ActivationFunctionType
AX = mybir.AxisListType

D = 1024
NH = 16
HD = 64
EPS = 1e-6
BIG = 30000.0
ENGS = ("pe", "act", "dve", "pool", "sp")


class Buf:
    __slots__ = ("name", "w", "r")

    def __init__(self, name):
        self.name = name
        self.w = {}
        self.r = {}


class Ctx:
    def __init__(self, nc):
        self.nc = nc
        self.q = {e: [] for e in ENGS}
        self.cnt = {"c_" + e: 0 for e in ENGS}
        self.waited = {e: {} for e in ENGS}
        self.dma_sems = []

    def _waits(self, eng, toks):
        need = {}
        for t in toks:
            sname, val, peng = t
            if peng == eng and eng == "pe":
                continue
            if self.waited[eng].get(sname, 0) >= val:
                continue
            if need.get(sname, 0) < val:
                need[sname] = val
        for sname, val in need.items():
            self.waited[eng][sname] = val
            self.q[eng].append(("wait", sname, val))

    @staticmethod
    def _deps(reads, writes):
        toks = []
        for b in reads:
            toks.extend(b.w.values())
        for b in writes:
            toks.extend(b.w.values())
            toks.extend(b.r.values())
        return toks

    def _record(self, tok, reads, writes):
        s = tok[0]
        for b in reads:
            b.r[s] = tok
        for b in writes:
            b.w[s] = tok

    def op(self, eng, fn, reads=(), writes=()):
        self._waits(eng, self._deps(reads, writes))
        sname = "c_" + eng
        self.cnt[sname] += 1
        tok = (sname, self.cnt[sname], eng)
        self.q[eng].append(("op", fn, sname, 1))
        self._record(tok, reads, writes)
        return tok

    def dma(self, eng, fn, sem, reads=(), writes=()):
        if sem not in self.cnt:
            self.cnt[sem] = 0
            self.dma_sems.append(sem)
        self._waits(eng, self._deps(reads, writes))
        self.cnt[sem] += 16
        tok = (sem, self.cnt[sem], "dma")
        self.q[eng].append(("op", fn, sem, 16))
        self._record(tok, reads, writes)
        return tok

    def barrier(self):
        toks = [("c_" + e, self.cnt["c_" + e], e) for e in ENGS if self.cnt["c_" + e] > 0]
        toks += [(s, self.cnt[s], "dma") for s in self.dma_sems if self.cnt[s] > 0]
        for e in ENGS:
            self._waits(e, [t for t in toks if t[0] != "c_" + e])

    def replay(self):
        nc = self.nc
        with ExitStack() as st:
            sems = {}
            for sname, n in self.cnt.items():
                if n > 0:
                    sems[sname] = st.enter_context(nc.semaphore(sname))
            block = st.enter_context(nc.Block())
            engobj = {"pe": block.tensor, "act": block.scalar, "dve": block.vector,
                      "pool": block.gpsimd, "sp": block.sync}

            def make(ename):
                def body(eng):
                    for item in self.q[ename]:
                        if item[0] == "wait":
                            eng.wait_ge(sems[item[1]], item[2])
                        else:
                            _, fn, sname, inc = item
                            fn(eng).then_inc(sems[sname], inc)
                return body

            for ename in ENGS:
                if self.q[ename]:
                    engobj[ename](make(ename))


def _bf16_round(a):
    a = np.asarray(a, np.float32)
    u = a.view(np.uint32).astype(np.uint64)
    u = (u + 0x7FFF + ((u >> 16) & 1)) & 0xFFFF0000
    return u.astype(np.uint32).view(np.float32)


def make_consts(S):
    NT = S // 128
    c = {}
    c["c_ident"] = np.eye(128, dtype=np.float32)
    bd = np.zeros((128, 128), np.float32)
    bd[:64, :64] = 1.0
    bd[64:, 64:] = 1.0
    c["c_bd1"] = bd
    c["c_bd64"] = bd / 64.0
    jr = np.arange(128)[:, None]
    tr = np.arange(128)[None, :]
    c["c_tri"] = np.where(tr >= jr, 0.0, -BIG).astype(np.float32)
    j = np.arange(S)
    ka = np.zeros((16, S), np.float32)
    for n in range(8):
        ka[n] = (j // 256 == n)
    ka[8] = ka[9] = j % 256
    ka[10] = ka[11] = j // 256
    ka[12] = 1.0
    c["c_kaug"] = ka
    slopes = (2.0 ** (-8.0 * np.arange(1, NH + 1) / NH)).astype(np.float32)
    qa = np.zeros((NH, 8, S), np.float32)
    for h in range(NH):
        s_hi = _bf16_round(slopes[h])
        s_lo = _bf16_round(np.float32(slopes[h] - s_hi))
        qa[h, 0] = s_hi
        qa[h, 1] = s_lo
        qa[h, 2] = 256.0 * s_hi
        qa[h, 3] = 256.0 * s_lo
        qa[h, 4] = _bf16_round(-(slopes[h] * j.astype(np.float32)))
    c["c_qaug"] = qa
    gc = np.zeros((NT, 2, 8), np.float32)
    for tt in range(NT):
        qb = tt // 2
        for n in range(8):
            gc[tt, :, n] = 0.0 if n < qb else (1e30 if n == qb else -1e30)
    c["c_gc"] = np.broadcast_to(gc.reshape(1, -1), (128, NT * 16)).copy()
    return c


def build_program(S=2048, NSEQ=2, layers=(0, 1), NHP=8, NFC=8, NWARM1=30, NWARM2=8):
    NT = S // 128
    NQ = S // 512
    nc = bass.Bass("TRN2", target_bir_lowering=False)

    def din(name, shape):
        return nc.dram_tensor(name, list(shape), F32, kind="ExternalInput").ap()

    x_d = din("x", [NSEQ, S, D])
    out_d = nc.dram_tensor("out", [NSEQ, S, D], F32, kind="ExternalOutput").ap()
    w_in_d = [din("w_in0", [D, 4 * D]), din("w_in1", [D, 4 * D])]
    w_out_d = [din("w_out0", [D, D]), din("w_out1", [D, D])]
    gb_d = din("gb", [2, 128, D])
    cw_d = din("cw", [128, 8, 3])
    gqk_d = din("gqk", [128, 2])
    ident_d = din("c_ident", [128, 128])
    bd1_d = din("c_bd1", [128, 128])
    bd64_d = din("c_bd64", [128, 128])
    tri_d = din("c_tri", [128, 128])
    kaug_d = din("c_kaug", [16, S])
    qaug_d = din("c_qaug", [NH, 8, S])
    gc_d = din("c_gc", [128, NT * 16])

    c = Ctx(nc)

    with ExitStack() as st:
        def T(name, shape, dt):
            return st.enter_context(nc.sbuf_tensor(name, list(shape), dt))

        h = T("h", [128, NT, D], F32)
        hnT = T("hnT", [128, 8, S], BF16)
        win = [T(f"win{i}", [128, 8, 4, 128], BF16) for i in range(2)]
        gb = T("gbs", [128, D], F32)
        gc = T("gcs", [128, NT * 16], F32)
        ident = T("ident", [128, 128], BF16)
        bd1 = T("bd1", [128, 128], BF16)
        bd64 = T("bd64", [128, 128], BF16)
        tri = T("tri", [128, 128], BF16)
        cw = T("cws", [128, 8, 3], F32)
        gqk = T("gqks", [128, 2], F32)
        hn_tok = [T(f"hn_tok{i}", [128, D], BF16) for i in range(2)]
        sqj = T("sqj", [128, D], BF16)
        ssq = T("ssq", [128, NT], F32)
        nrm = T("nrm", [128, NT], F32)
        rstdN = T("rstdN", [128, NT], F32)

        ARENA = 76 * 1024
        arena = T("arena", [128, ARENA // 2], BF16)
        apos = [0]

        def carve_reset():
            apos[0] = 0

        def carve(shape_free, dt):
            n = int(np.prod(shape_free))
            nb = n * (4 if dt == F32 else 2)
            a = apos[0]
            assert a % 4 == 0
            apos[0] = a + ((nb + 3) // 4) * 4
            assert apos[0] <= ARENA, ("arena overflow", apos[0])
            v = arena[:, a // 2:(a + nb) // 2]
            if dt == F32:
                v = v.bitcast(F32)
            if len(shape_free) == 2:
                v = v.rearrange("p (a b) -> p a b", b=shape_free[1])
            elif len(shape_free) == 3:
                v = v.rearrange("p (a b c) -> p a b c", b=shape_free[1], c=shape_free[2])
            return v

        carve_reset()
        yT = carve([8, S], BF16)
        wout0 = carve([8, D], BF16)
        uf = carve([S + 2], F32)
        c_sb = [carve([512], F32) for _ in range(2)]
        sz = [carve([512], F32) for _ in range(2)]
        gg = [carve([512], F32) for _ in range(2)]
        acc = [carve([512], F32) for _ in range(2)]
        l0_bytes = apos[0]
        carve_reset()
        QA = carve([S], BF16)
        QB = carve([S], BF16)
        KA = carve([S], BF16)
        KB = carve([S], BF16)
        VA = carve([NT, 128], BF16)
        VB = carve([NT, 128], BF16)
        gz = carve([S], BF16)
        yTp2 = [carve([S], BF16) for _ in range(2)]
        pT = [carve([512], BF16) for _ in range(4)]
        sq = [carve([512], BF16) for _ in range(6)]
        lnv = [carve([512], F32) for _ in range(2)]
        rstd = lnv
        ez = [carve([512], F32) for _ in range(2)]

        rc = [carve([512], F32) for _ in range(2)]
        t1 = [carve([512], F32) for _ in range(2)]
        t1b = [carve([512], F32)] * 2
        Gt = carve([NT * 16], F32)
        M8 = carve([NT * 16], F32)
        AL = carve([NT * 16], F32)
        MB = carve([NT * 16], BF16)
        kms = carve([8], F32)
        km = carve([8], BF16)
        wout1 = [carve([D], BF16) for _ in range(4)]
        l1_bytes = apos[0]
        print("arena bytes: l0", l0_bytes, "l1", l1_bytes)

        psum = st.enter_context(nc.psum_tensor("psum", [128, 4096], F32))

        def bank(b):
            return psum[:, b * 512:(b + 1) * 512]

        def bank_bf(b, nb=1):
            return psum[:, b * 512:(b + nb) * 512].bitcast(BF16)

        B = lambda n: Buf(n)
        hB2 = [[B(f"h{t}_{k}") for k in range(2)] for t in range(NT)]
        hnTB = [B(f"hnT{t}") for t in range(NT)]
        winB = [B("win0"), B("win1")]
        constB = B("const")
        gbB = B("gb")
        hn_tokB = [B("hn_tok0"), B("hn_tok1")]
        sqjB, ssqB, nrmB, rstdNB = B("sqj"), B("ssq"), B("nrm"), B("rstdN")
        pB = [B(f"bank{i}") for i in range(8)]
        yTB = [B(f"yT{t}") for t in range(NT)]
        wout0B = B("wout0")
        ufB = [B(f"uf{t}") for t in range(NQ)]
        c_sbB, szB, ggB, accB = ([B(f"{n}{i}") for i in range(2)] for n in ("c_sb", "sz", "gg", "acc"))
        QAd = [B(f"QAd{t}") for t in range(NQ)]
        QBd = [B(f"QBd{t}") for t in range(NQ)]
        KAd = [B(f"KAd{t}") for t in range(NQ)]
        KBd = [B(f"KBd{t}") for t in range(NQ)]
        QAm, QBm, QAc, QBc, KAc, KBc = (B(n) for n in ("QAm", "QBm", "QAc", "QBc", "KAc", "KBc"))
        VAd = [B(f"VAd{t}") for t in range(NQ)]
        VBd = [B(f"VBd{t}") for t in range(NQ)]
        gzB = [B(f"gz{t}") for t in range(NQ)]
        yTpB2 = [[B(f"yTp{i}_{t}") for t in range(NQ)] for i in range(2)]
        pTB = [B(f"pT{i}") for i in range(4)]
        lnvB, ezB, rcB, t1B = ([B(f"{n}{i}") for i in range(2)] for n in ("lnv", "ez", "rc", "t1"))
        sqB = [B(f"sq{i}") for i in range(6)]
        rstdB = lnvB
        ez2B = t1B
        ez2 = t1
        t1bB = [B("t1b")] * 2
        GtB, M8B, ALB, MBB, kmsB, kmB = (B(n) for n in ("Gt", "M8", "AL", "MB", "kms", "km"))
        wout1B = [B(f"wout1_{i}") for i in range(4)]

        def MM(out, lhsT, rhs, start, stop, reads, writes):
            return c.op("pe", lambda e: e.matmul(out, lhsT=lhsT, rhs=rhs, start=start, stop=stop), reads, writes)

        def TR(out, in_, reads, writes):
            return c.op("pe", lambda e: e.transpose(out, in_, ident[:, :]), list(reads) + [constB], writes)

        def ACT(out, in_, func, reads, writes, bias=None, scale=None, accum_out=None):
            kw = {}
            if bias is not None:
                kw["bias"] = bias
            if scale is not None:
                kw["scale"] = scale
            if accum_out is not None:
                kw["accum_out"] = accum_out
            return c.op("act", lambda e: e.activation(out, in_, func, **kw), reads, writes)

        def TT(out, in0, in1, op, reads, writes, eng="dve"):
            return c.op(eng, lambda e: e.tensor_tensor(out=out, in0=in0, in1=in1, op=op), reads, writes)

        def TS(out, in0, s1, s2, op0, op1, reads, writes, eng="dve"):
            if op1 is None:
                return c.op(eng, lambda e: e.tensor_scalar(out=out, in0=in0, scalar1=s1, scalar2=None, op0=op0), reads, writes)
            return c.op(eng, lambda e: e.tensor_scalar(out=out, in0=in0, scalar1=s1, scalar2=s2, op0=op0, op1=op1), reads, writes)

        def STT(out, in0, scalar, in1, op0, op1, reads, writes):
            return c.op("dve", lambda e: e.scalar_tensor_tensor(out=out, in0=in0, scalar=scalar, in1=in1, op0=op0, op1=op1), reads, writes)

        def CP(eng, out, in_, reads, writes):
            if eng == "act":
                return c.op("act", lambda e: e.activation(out, in_, AF.Copy), reads, writes)
            return c.op(eng, lambda e: e.tensor_copy(out=out, in_=in_), reads, writes)

        def RCP(out, in_, reads, writes):
            return c.op("dve", lambda e: e.reciprocal(out=out, in_=in_), reads, writes)

        def MEMSET(eng, ap, val, writes):
            return c.op(eng, lambda e: e.memset(ap, val), (), writes)

        def DMA(eng, out, in_, sem, reads, writes):
            return c.dma(eng, lambda e: e.dma_start(out=out, in_=in_), sem, reads, writes)

        for (dst, src) in ((ident, ident_d), (bd1, bd1_d), (bd64, bd64_d), (tri, tri_d)):
            DMA("pool", dst[:, :], src, "d_setup_p", (), [constB])
        DMA("sp", gc[:, :], gc_d, "d_setup", (), [constB])
        DMA("sp", cw[:, :, :], cw_d, "d_setup", (), [constB])
        DMA("sp", gqk[:, :], gqk_d, "d_setup", (), [constB])
        c.barrier()

        units = []
        for s in range(NSEQ):
            if 0 in layers:
                units += [(s, 0, fc) for fc in range(NFC)]
            if 1 in layers:
                units += [(s, 1, hp) for hp in range(NHP)]
        unit_idx = {u: i for i, u in enumerate(units)}

        def load_unit(i):
            if i >= len(units):
                return
            s, L, j = units[i]
            slot = i % 2
            src = w_in_d[L].rearrange("(kc p) (seg f m) -> p kc seg f m", p=128, seg=4, f=8, m=128)
            for seg in range(4):
                DMA("pool", win[slot][:, :, seg, :], src[:, :, seg, j, :], f"d_win{slot}", (), [winB[slot]])
            if L == 1 and j > 0:
                DMA("pool", wout1[j % 4][:, :], w_out_d[1][j * 128:(j + 1) * 128, :], f"d_wo1_{j % 4}", (), [wout1B[j % 4]])

        def norm_phase(s, L, load_x):
            DMA("sp", gb[:, :], gb_d[L], "d_gb", (), [gbB])
            for tt in range(NT):
                if load_x:
                    DMA("sp", h[:, tt, :], x_d[s, tt * 128:(tt + 1) * 128, :], f"d_x{tt}", (), hB2[tt])
                if tt % 2 == 0:
                    ACT(sqj[:, :], h[:, tt, :], AF.Square, hB2[tt], [sqjB, ssqB], accum_out=ssq[:, tt:tt + 1])
                else:
                    c.op("dve", lambda e, tt=tt: e.scalar_tensor_tensor(
                        out=hn_tok[1][:, :], in0=h[:, tt, :], scalar=1.0, in1=h[:, tt, :], op0=ALU.mult, op1=ALU.mult,
                        accum_out=ssq[:, tt:tt + 1]), hB2[tt], [hn_tokB[1], ssqB])
            TS(nrm[:, :], ssq[:, :], 1.0 / D, EPS, ALU.mult, ALU.add, [ssqB], [nrmB])
            ACT(nrm[:, :], nrm[:, :], AF.Ln, [nrmB], [nrmB])
            ACT(rstdN[:, :], nrm[:, :], AF.Exp, [nrmB], [rstdNB], scale=-0.5)
            for tt in range(NT):
                b = tt % 2
                STT(hn_tok[b][:, :], h[:, tt, :], rstdN[:, tt:tt + 1], gb[:, :], ALU.mult, ALU.mult,
                    hB2[tt] + [rstdNB, gbB], [hn_tokB[b]])
                pb = 6 + (tt % 2)
                pv = bank_bf(pb)
                for kc in range(8):
                    TR(pv[:, kc * 128:(kc + 1) * 128], hn_tok[b][:, kc * 128:(kc + 1) * 128],
                       [hn_tokB[b]], [pB[pb]])
                CP("act", hnT[:, :, tt * 128:(tt + 1) * 128], pv.rearrange("p (k t) -> p k t", t=128),
                   [pB[pb]], [hnTB[tt]])

        def layer0(s):
            MEMSET("dve", uf[:, 0:2], 0.0, [ufB[0]])
            DMA("pool", wout0[:, :, :], w_out_d[0].rearrange("(kc p) n -> p kc n", p=128), "d_wo0", (), [wout0B])
            it = 0
            for fc in range(NFC):
                ui = unit_idx[(s, 0, fc)]
                slot = ui % 2
                load_unit(ui + 1)
                w = win[slot]
                for tq in range(NQ):
                    b = it % 2
                    it += 1
                    tsl = slice(tq * 512, (tq + 1) * 512)
                    hr = [hnTB[t] for t in range(tq * 4, tq * 4 + 4)]
                    pbase = 4 * b
                    for seg in (1, 2, 3, 0):
                        pb = pbase + seg
                        for kc in range(8):
                            MM(bank(pb), w[:, kc, seg, :], hnT[:, kc, tsl], kc == 0, kc == 7,
                               [winB[slot]] + hr, [pB[pb]])
                    CP("act", c_sb[b][:, :], bank(pbase + 1), [pB[pbase + 1]], [c_sbB[b]])
                    TT(uf[:, 2 + tq * 512:2 + (tq + 1) * 512], c_sb[b][:, :], bank(pbase + 2), ALU.mult,
                       [c_sbB[b], pB[pbase + 2]], [ufB[tq]])
                    ur = [ufB[tq]] + ([ufB[tq - 1]] if tq > 0 else [])
                    t0 = tq * 512
                    TS(acc[b][:, :], uf[:, t0 + 2:t0 + 514], cw[:, fc, 2:3], None, ALU.mult, None,
                       ur + [constB], [accB[b]])
                    STT(acc[b][:, :], uf[:, t0 + 1:t0 + 513], cw[:, fc, 1:2], acc[b][:, :], ALU.mult, ALU.add,
                        ur + [constB, accB[b]], [accB[b]])
                    STT(acc[b][:, :], uf[:, t0:t0 + 512], cw[:, fc, 0:1], acc[b][:, :], ALU.mult, ALU.add,
                        ur + [constB, accB[b]], [accB[b]])
                    ACT(sz[b][:, :], bank(pbase + 3), AF.Silu, [pB[pbase + 3]], [szB[b]])
                    TT(gg[b][:, :], bank(pbase + 0), sz[b][:, :], ALU.mult, [pB[pbase + 0], szB[b]], [ggB[b]])
                    TT(yT[:, fc, tsl], acc[b][:, :], gg[b][:, :], ALU.mult, [accB[b], ggB[b]],
                       [yTB[t] for t in range(tq * 4, tq * 4 + 4)])
            it = 0
            for tt in range(NT):
                for half in range(2):
                    pb = it % 4
                    it += 1
                    for kc in range(NFC):
                        MM(bank(pb), yT[:, kc, tt * 128:(tt + 1) * 128], wout0[:, kc, half * 512:(half + 1) * 512],
                           kc == 0, kc == NFC - 1, [yTB[tt], wout0B], [pB[pb]])
                    hs = h[:, tt, half * 512:(half + 1) * 512]
                    TT(hs, hs, bank(pb), ALU.add, [hB2[tt][half], pB[pb]], [hB2[tt][half]])

        def layer1_setup():
            MEMSET("dve", QB[0:64, :], 0.0, [QBm, QBc] + QBd)
            MEMSET("dve", KB[0:64, :], 0.0, [KBc] + KBd)
            MEMSET("dve", VA[:, :, :], 1.0, VAd)
            MEMSET("dve", VB[:, :, :], 1.0, VBd)
            MEMSET("dve", kms[:, :], 0.0, [kmsB])
            r2 = lambda ap: ap.rearrange("r (a b) -> r a b", b=512)
            DMA("pool", r2(KA[64:80, :]), r2(kaug_d), "d_kaugA", (), [KAc])
            DMA("pool", r2(KB[0:16, :]), r2(kaug_d), "d_kaugB", (), [KBc])

        def layer1(s):
            layer1_setup()
            DMA("pool", wout1[0][:, :], w_out_d[1][0:128, :], "d_wo1_0", (), [wout1B[0]])
            it = 0
            oit = 0

            for hp in range(NHP):
                ui = unit_idx[(s, 1, hp)]
                slot = ui % 2
                load_unit(ui + 1)
                w = win[slot]
                r2 = lambda ap: ap.rearrange("r (a b) -> r a b", b=512)
                DMA("pool", r2(QB[8:16, :]), r2(qaug_d[2 * hp + 1]), "d_qaugB", (), [QBc])
                def stageA(tq):
                    tsl = slice(tq * 512, (tq + 1) * 512)
                    hr = [hnTB[t] for t in range(tq * 4, tq * 4 + 4)]
                    qb_, kb_ = QKSETS[tq % 3]
                    for (seg, pb) in ((0, qb_), (1, kb_)):
                        for kc in range(8):
                            MM(bank(pb), w[:, kc, seg, :], hnT[:, kc, tsl], kc == 0, kc == 7,
                               [winB[slot]] + hr, [pB[pb]])
                    i0 = (tq % 3) * 2
                    ACT(sq[i0][:, :], bank(qb_), AF.Square, [pB[qb_]], [sqB[i0]])
                    ACT(sq[i0 + 1][:, :], bank(kb_), AF.Square, [pB[kb_]], [sqB[i0 + 1]])

                def stageB(tq):
                    tsl = slice(tq * 512, (tq + 1) * 512)
                    qb_, kb_ = QKSETS[tq % 3]
                    i0 = (tq % 3) * 2
                    MM(bank(0), bd1[:, :], sq[i0][:, :], True, True, [constB, sqB[i0]], [pB[0]])
                    MM(bank(1), bd64[:, :], sq[i0 + 1][:, :], True, True, [constB, sqB[i0 + 1]], [pB[1]])
                    ACT(lnv[0][:, :], bank(0), AF.Ln, [pB[0]], [lnvB[0]], bias=float(HD * EPS))
                    ACT(rstd[0][:, :], lnv[0][:, :], AF.Exp, [lnvB[0]], [rstdB[0]], scale=-0.5)
                    STT(QA[:, tsl], bank(qb_), gqk[:, 0:1], rstd[0][:, :], ALU.mult, ALU.mult,
                        [pB[qb_], constB, rstdB[0]], [QAd[tq], QAm, QAc])
                    CP("dve", QB[64:128, tsl], QA[64:128, tsl], [QAd[tq]], [QBd[tq]])
                    ACT(lnv[1][:, :], bank(1), AF.Ln, [pB[1]], [lnvB[1]], bias=float(EPS))
                    ACT(rstd[1][:, :], lnv[1][:, :], AF.Exp, [lnvB[1]], [rstdB[1]], scale=-0.5)
                    STT(KA[:, tsl], bank(kb_), gqk[:, 1:2], rstd[1][:, :], ALU.mult, ALU.mult,
                        [pB[kb_], constB, rstdB[1]], [KAd[tq], KAc])
                    c.op("dve", lambda e, tq=tq, tsl=tsl: e.tensor_reduce(
                        out=kms[:, 2 * tq:2 * tq + 2], in_=KA[:, tsl].rearrange("p (n k) -> p n k", k=256),
                        axis=AX.X, op=ALU.add), [KAd[tq]], [kmsB])
                    CP("dve", KB[64:128, tsl], KA[64:128, tsl], [KAd[tq]], [KBd[tq]])

                QKSETS = ((5, 6), (3, 4), (7, 2))

                def stageC_pe_act(tq):
                    b = tq % 2
                    tsl = slice(tq * 512, (tq + 1) * 512)
                    hr = [hnTB[t] for t in range(tq * 4, tq * 4 + 4)]
                    zb, vb = QKSETS[(NQ + tq) % 3]
                    for kc in range(8):
                        MM(bank(zb), w[:, kc, 3, :], hnT[:, kc, tsl], kc == 0, kc == 7, [winB[slot]] + hr, [pB[zb]])
                    for t4 in range(4):
                        tt = tq * 4 + t4
                        for kc in range(8):
                            MM(bank(vb)[:, t4 * 128:(t4 + 1) * 128], hnT[:, kc, tt * 128:(tt + 1) * 128], w[:, kc, 2, :],
                               kc == 0, kc == 7, [winB[slot], hnTB[tt]], [pB[vb]])
                    ACT(ez[b][:, :], bank(zb), AF.Exp, [pB[zb]], [ezB[b]], scale=-1.0)
                    ACT(ez[b][:, :], ez[b][:, :], AF.Ln, [ezB[b]], [ezB[b]], bias=1.0)
                    ACT(ez2[b][:, :], ez[b][:, :], AF.Exp, [ezB[b]], [ez2B[b]], scale=-1.0)

                def stageC_dve(tq):
                    b = tq % 2
                    tsl = slice(tq * 512, (tq + 1) * 512)
                    zb, vb = QKSETS[(NQ + tq) % 3]
                    TT(gz[:, tsl], bank(zb), ez2[b][:, :], ALU.mult, [pB[zb], ez2B[b]], [gzB[tq]])
                    pv3 = bank(vb).rearrange("p (t c) -> p t c", c=128)
                    CP("dve", VA[:, tq * 4:(tq + 1) * 4, 0:64], pv3[:, :, 0:64], [pB[vb]], [VAd[tq]])
                    CP("dve", VB[:, tq * 4:(tq + 1) * 4, 0:64], pv3[:, :, 64:128], [pB[vb]], [VBd[tq]])

                n_early_c = 0
                for step in range(NQ + 1):
                    cnow = None
                    if step < NQ:
                        stageA(step)
                    elif step - NQ < NQ:
                        cnow = step - NQ
                        stageC_pe_act(cnow)
                        n_early_c += 1
                    if step >= 1:
                        stageB(step - 1)
                    if cnow is not None:
                        stageC_dve(cnow)
                if n_early_c < NQ:
                    stageC_pe_act(n_early_c)
                    pending_c_dve = [n_early_c]
                    n_early_c += 1
                else:
                    pending_c_dve = []
                DMA("pool", r2(QA[72:80, :]), r2(qaug_d[2 * hp]), "d_qaugA", (), [QAc] + QAd)
                DMA("pool", r2(KA[64:80, :]), r2(kaug_d), "d_kaugA", (), [KAc] + KAd)
                TS(km[:, :], kms[:, :], 1.0 / 256.0, None, ALU.mult, None, [kmsB], [kmB])
                for tt in range(NT):
                    tq = tt // 4
                    for hh in range(2):
                        g = tt * 2 + hh
                        if hh == 0:
                            MM(bank(0)[:, g * 8:(g + 1) * 8], QA[0:64, tt * 128:(tt + 1) * 128], km[0:64, :],
                               True, True, [QAd[tq], kmB], [pB[0]])
                        else:
                            MM(bank(0)[:, g * 8:(g + 1) * 8], QB[64:128, tt * 128:(tt + 1) * 128], km[64:128, :],
                               True, True, [QBd[tq], kmB], [pB[0]])
                NG = NT * 2
                TT(Gt[:, :], bank(0)[:, 0:NG * 8], gc[:, :], ALU.add, [pB[0], constB], [GtB])
                late = [("dve", k) for k in pending_c_dve] + [("all", k) for k in range(n_early_c, NQ)]
                for g in range(NG // 2):
                    c.op("dve", lambda e, g=g: e.max(out=M8[:, g * 8:(g + 1) * 8], in_=Gt[:, g * 8:(g + 1) * 8]),
                         [GtB], [M8B])
                for kind, k in late:
                    if kind == "dve":
                        stageC_dve(k)
                for kind, k in late:
                    if kind == "all":
                        stageC_pe_act(k)
                for g in range(NG // 2, NG):
                    c.op("dve", lambda e, g=g: e.max(out=M8[:, g * 8:(g + 1) * 8], in_=Gt[:, g * 8:(g + 1) * 8]),
                         [GtB], [M8B])
                G3 = Gt[:, :].rearrange("p (g e) -> p g e", e=8)
                thr = M8[:, :].rearrange("p (g e) -> p g e", e=8)[:, :, 3:4].to_broadcast([128, NG, 8])
                TT(AL[:, :].rearrange("p (g e) -> p g e", e=8), G3, thr, ALU.is_ge, [GtB, M8B], [ALB])
                TS(MB[:, :], AL[:, :], -1.0, BIG, ALU.add, ALU.mult, [ALB], [MBB])
                mbT = bank_bf(0, 2)
                for tt in range(NT):
                    for hh in range(2):
                        g = tt * 2 + hh
                        base = 64 if hh == 0 else 0
                        TR(mbT[base:base + 8, tt * 128:(tt + 1) * 128], MB[:, g * 8:(g + 1) * 8],
                           [MBB], [pB[(tt * 128) // 1024]])
                for half in range((S + 1023) // 1024):
                    lo, hi = half * 1024, min(S, (half + 1) * 1024)
                    CP("dve", QA[64:72, lo:hi], mbT[64:72, lo:hi], [pB[half]], [QAm])
                    CP("act", QB[0:8, lo:hi], mbT[0:8, lo:hi], [pB[half]], [QBm])
                for kind, k in late:
                    if kind == "all":
                        stageC_dve(k)
                yTp = yTp2[hp % 2]
                yTpB = yTpB2[hp % 2]
                selA = (QA, KA, VA, slice(0, 80), QAd, QAm, QAc, KAd, KAc, VAd)
                selB = (QB, KB, VB, slice(0, 128), QBd, QBm, QBc, KBd, KBc, VBd)
                tiles = []
                for hh in range(2):
                    for qt in range(NQ):
                        nkc = 4 * qt + 4
                        for kc in range(nkc):
                            tiles.append((hh, qt, kc, nkc, it % 4))
                            it += 1
                LA = 3
                SBANK = (0, 1, 2, 7)
                grp = {}

                def front(i):
                    hh, qt, kc, nkc, sb = tiles[i]
                    Qt, Kt, Vt, kr, Qd, Qm, Qc, Kd, Kc, Vd = selA if hh == 0 else selB
                    dd = kc - 4 * qt
                    lo = 128 * dd if dd > 0 else 0
                    bk = SBANK[sb]
                    MM(bank(bk)[:, lo:512], Kt[kr, kc * 128:(kc + 1) * 128],
                       Qt[kr, qt * 512 + lo:(qt + 1) * 512], True, dd < 0,
                       [Kd[kc // 4], Kc, Qd[qt], Qm, Qc], [pB[bk]])
                    if dd >= 0:
                        MM(bank(bk)[:, lo:lo + 128], ident[:, :], tri[:, :], False, True, [constB], [pB[bk]])
                    ACT(pT[sb][:, lo:512], bank(bk)[:, lo:512], AF.Exp, [pB[bk]], [pTB[sb]])

                def back(i):
                    nonlocal oit
                    hh, qt, kc, nkc, sb = tiles[i]
                    Qt, Kt, Vt, kr, Qd, Qm, Qc, Kd, Kc, Vd = selA if hh == 0 else selB
                    dd = kc - 4 * qt
                    lo = 128 * dd if dd > 0 else 0
                    if kc == 0:
                        grp[(hh, qt)] = (3 + (oit % 4), oit % 2)
                        oit += 1
                    ob, ob2 = grp[(hh, qt)]
                    MM(bank(ob)[:, lo:512], Vt[:, kc, :], pT[sb][:, lo:512], kc == 0, kc == nkc - 1,
                       [Vd[kc // 4], pTB[sb]], [pB[ob]])
                    if kc == nkc - 1:
                        qsl = slice(qt * 512, (qt + 1) * 512)
                        RCP(rc[ob2][64:128, :], bank(ob)[64:128, :], [pB[ob]], [rcB[ob2]])
                        TT(t1[ob2][0:64, :], bank(ob)[0:64, :], rc[ob2][64:128, :], ALU.mult,
                           [pB[ob], rcB[ob2]], [t1B[ob2]])
                        if hh == 0:
                            TT(yTp[0:64, qsl], t1[ob2][0:64, :], gz[0:64, qsl], ALU.mult,
                               [t1B[ob2], gzB[qt]], [yTpB[qt]], eng="pool")
                        else:
                            CP("dve", t1b[ob2][64:128, :], t1[ob2][0:64, :], [t1B[ob2]], [t1bB[ob2]])
                            TT(yTp[64:128, qsl], t1b[ob2][64:128, :], gz[64:128, qsl], ALU.mult,
                               [t1bB[ob2], gzB[qt]], [yTpB[qt]], eng="pool")

                for i in range(len(tiles) + LA):
                    if i < len(tiles):
                        front(i)
                    if i - LA >= 0:
                        back(i - LA)
                if hp % 2 == 1 or hp == NHP - 1:
                    pairs = [hp - 1, hp] if hp % 2 == 1 else [hp]
                    for tt in range(NT):
                        for half in range(2):
                            pb = 5 + (it % 3)
                            it += 1
                            for idx, ph in enumerate(pairs):
                                MM(bank(pb), yTp2[ph % 2][:, tt * 128:(tt + 1) * 128],
                                   wout1[ph % 4][:, half * 512:(half + 1) * 512],
                                   idx == 0, idx == len(pairs) - 1,
                                   [yTpB2[ph % 2][tt // 4], wout1B[ph % 4]], [pB[pb]])
                            hs = h[:, tt, half * 512:(half + 1) * 512]
                            if (tt * 2 + half) % 3 == 2:
                                sb_ = (tt * 2 + half) % 2
                                CP("act", t1[sb_][:, :], bank(pb), [pB[pb]], [t1B[sb_]])
                                TT(hs, hs, t1[sb_][:, :], ALU.add, [hB2[tt][half], t1B[sb_]], [hB2[tt][half]], eng="pool")
                            else:
                                TT(hs, hs, bank(pb), ALU.add, [hB2[tt][half], pB[pb]], [hB2[tt][half]])

        load_unit(0)
        for s in range(NSEQ):
            norm_phase(s, layers[0], load_x=True)
            for li, L in enumerate(layers):
                if L == 0:
                    layer0(s)
                else:
                    layer1(s)
                if li + 1 < len(layers):
                    norm_phase(s, layers[li + 1], load_x=False)
                    c.barrier()
            for tt in range(NT):
                DMA("sp", out_d[s, tt * 128:(tt + 1) * 128, :], h[:, tt, :], "d_out", hB2[tt], ())
            c.barrier()
        c.replay()
    return nc


def _common_inputs(norm_g, conv_w_in, conv_w, conv_w_out, attn_w_in, q_norm_g, k_norm_g, attn_w_out, S):
    f = lambda a: np.ascontiguousarray(np.asarray(a, dtype=np.float32))
    m = {
        "w_in0": f(conv_w_in[0]), "w_out0": f(conv_w_out[0]),
        "w_in1": f(attn_w_in[0]), "w_out1": f(attn_w_out[0]),
        "gb": f(np.broadcast_to(np.asarray(norm_g)[:, None, :], (2, 128, D))),
        "cw": f(np.asarray(conv_w[0]).T.reshape(8, 128, 3).transpose(1, 0, 2)),
        "gqk": f(np.stack([np.tile(np.asarray(q_norm_g[0]), 2), np.tile(np.asarray(k_norm_g[0]), 2)], axis=1)),
    }
    m.update(make_consts(S))
    return m


_PROG_CACHE = {}


def _get_prog(key, **kw):
    if key not in _PROG_CACHE:
        _PROG_CACHE[key] = build_program(**kw)
    return _PROG_CACHE[key]


FUSED = True


def kernel(x, norm_g, conv_w_in, conv_w, conv_w_out, attn_w_in, q_norm_g, k_norm_g, attn_w_out):
    x = np.asarray(x, dtype=np.float32)
    Bsz, S, _ = x.shape
    n = 8
    nseq = Bsz // n
    common = _common_inputs(norm_g, conv_w_in, conv_w, conv_w_out, attn_w_in, q_norm_g, k_norm_g, attn_w_out, S)
    shards = [np.ascontiguousarray(x[i * nseq:(i + 1) * nseq]) for i in range(n)]
    if FUSED:
        stages = [(0, 1)]
    else:
        stages = [(0,), (1,)]
    for layers in stages:
        nc = _get_prog((S, nseq, layers), S=S, NSEQ=nseq, layers=layers)
        in_maps = [dict(common, x=shards[i]) for i in range(n)]
        res = run_bass_kernel_spmd(nc, in_maps, core_ids=list(range(n)))
        shards = [np.ascontiguousarray(np.asarray(res.results[i]["out"], dtype=np.float32)) for i in range(n)]
    return np.concatenate(shards, axis=0).astype(np.float32)
```
